# Optimizing a Trainium2 kernel written in Bass

```python
import math
import jax, jax.numpy as jnp
from jax import lax
import numpy as np

D_MODEL = 1024
BATCH = 4
SEQ = 4096
DEPTH = 1

CHUNK = 64
CONV_W = 4
EPS = 1e-6

DN_HEADS = 4
DN_DK = 128
DN_DV = 128
DN_QK = DN_HEADS * DN_DK
DN_V = DN_HEADS * DN_DV
DN_CONV_CH = 2 * DN_QK + DN_V

ML_HEADS = 4
ML_DK = 64
ML_DV = 128
ML_QK = ML_HEADS * ML_DK
ML_V = ML_HEADS * ML_DV

D_MIX = DN_V + ML_V

OFF_DN_QKV = 0
OFF_DN_Z = OFF_DN_QKV + DN_CONV_CH
OFF_DN_B = OFF_DN_Z + DN_V
OFF_DN_A = OFF_DN_B + DN_HEADS
OFF_ML_QK = OFF_DN_A + DN_HEADS
OFF_ML_V = OFF_ML_QK + 2 * ML_QK
OFF_ML_O = OFF_ML_V + ML_V
OFF_ML_I = OFF_ML_O + ML_V
OFF_ML_F = OFF_ML_I + ML_HEADS
D_IN = OFF_ML_F + ML_HEADS

N_EXPERTS = 32
TOP_K = 4
D_FF = D_MODEL
SWIGLU_LIMIT = 7.0
SWIGLU_ALPHA = 1.702
MOE_BLOCK = 256

kernel_name = 'hymba_gdn_mlstm_moe_adaln_block'


def rms_norm(x, w):
    xf = x.astype(jnp.float32)
    y = xf * lax.rsqrt(jnp.mean(xf * xf, axis=-1, keepdims=True) + EPS)
    return (y * w.astype(jnp.float32)).astype(x.dtype)


def modulate(h, shift, scale):
    return h * (1.0 + scale) + shift


def l2_normalize(u):
    return u * lax.rsqrt(jnp.sum(u * u, axis=-1, keepdims=True) + EPS)


def causal_conv_silu(u, w):
    S = u.shape[1]
    up = jnp.pad(u, ((0, 0), (CONV_W - 1, 0), (0, 0)))
    y = sum(up[:, j:j + S] * w[j] for j in range(CONV_W))
    return jax.nn.silu(y)


def to_chunks(u, n_heads):
    B, S, W = u.shape
    return u.reshape(B, S // CHUNK, CHUNK, n_heads, W // n_heads).transpose(0, 3, 1, 2, 4)


def gate_chunks(u):
    B, S, H = u.shape
    return u.reshape(B, S // CHUNK, CHUNK, H).transpose(0, 3, 1, 2)


def from_chunks(u):
    B, H, NC, C, d = u.shape
    return u.transpose(0, 2, 3, 1, 4).reshape(B, NC * C, H, d)


def gated_deltanet(proj, conv_w, a_log, dt_bias, norm_w):
    B, S, _ = proj.shape
    qkv = causal_conv_silu(proj[..., OFF_DN_QKV:OFF_DN_Z], conv_w)
    q = l2_normalize(to_chunks(qkv[..., :DN_QK], DN_HEADS)) * (DN_DK ** -0.5)
    k = l2_normalize(to_chunks(qkv[..., DN_QK:2 * DN_QK], DN_HEADS))
    v = to_chunks(qkv[..., 2 * DN_QK:], DN_HEADS)
    beta = gate_chunks(jax.nn.sigmoid(proj[..., OFF_DN_B:OFF_DN_A]))
    g = gate_chunks(-jnp.exp(a_log) * jax.nn.softplus(proj[..., OFF_DN_A:OFF_ML_QK] + dt_bias))
    G = jnp.cumsum(g, axis=-1)
    incl = jnp.tril(jnp.ones((CHUNK, CHUNK), bool))
    strict = jnp.tril(jnp.ones((CHUNK, CHUNK), bool), k=-1)
    decay = jnp.exp(jnp.where(incl, G[..., :, None] - G[..., None, :], -jnp.inf))
    kk = jnp.einsum('bhncd,bhnsd->bhncs', k, k)
    lower = jnp.where(strict, beta[..., :, None] * kk * decay, 0.0)
    unit_lower = jnp.eye(CHUNK, dtype=lower.dtype) + lower
    rhs = jnp.concatenate([v * beta[..., None], k * (beta * jnp.exp(G))[..., None]], axis=-1)
    sol = lax.linalg.triangular_solve(unit_lower, rhs, left_side=True, lower=True,
                                      unit_diagonal=True)
    w_val, k_cum = sol[..., :DN_DV], sol[..., DN_DV:]
    qk = jnp.einsum('bhncd,bhnsd->bhncs', q, k) * decay
    G_last = G[..., -1:]
    q_dec = q * jnp.exp(G)[..., None]
    k_dec = k * jnp.exp(G_last - G)[..., None]
    s_dec = jnp.exp(G_last[..., 0])

    def step(state, xs):
        w_c, kc_c, qk_c, q_c, k_c, sd_c = xs
        v_new = w_c - jnp.einsum('bhcd,bhde->bhce', kc_c, state)
        o = jnp.einsum('bhcd,bhde->bhce', q_c, state) + jnp.einsum('bhcs,bhse->bhce', qk_c, v_new)
        state = sd_c[..., None, None] * state + jnp.einsum('bhsd,bhse->bhde', k_c, v_new)
        return state, o

    xs = tuple(jnp.moveaxis(t, 2, 0) for t in (w_val, k_cum, qk, q_dec, k_dec, s_dec))
    state0 = jnp.zeros((B, DN_HEADS, DN_DK, DN_DV), jnp.float32)
    _, o = lax.scan(step, state0, xs)
    o = from_chunks(jnp.moveaxis(o, 0, 2))
    z = proj[..., OFF_DN_Z:OFF_DN_B].reshape(B, S, DN_HEADS, DN_DV)
    o = rms_norm(o, norm_w) * jax.nn.silu(z)
    return o.reshape(B, S, DN_V)


def mlstm(proj, conv_w, i_bias, f_bias, norm_w):
    B, S, _ = proj.shape
    qk_in = causal_conv_silu(proj[..., OFF_ML_QK:OFF_ML_V], conv_w)
    q = to_chunks(qk_in[..., :ML_QK], ML_HEADS) * (ML_DK ** -0.5)
    k = to_chunks(qk_in[..., ML_QK:], ML_HEADS)
    v = to_chunks(proj[..., OFF_ML_V:OFF_ML_O], ML_HEADS)
    i_pre = gate_chunks(proj[..., OFF_ML_I:OFF_ML_F] + i_bias)
    log_f = gate_chunks(jax.nn.log_sigmoid(proj[..., OFF_ML_F:D_IN] + f_bias))
    b = jnp.cumsum(log_f, axis=-1)
    incl = jnp.tril(jnp.ones((CHUNK, CHUNK), bool))
    d_mat = jnp.where(incl, b[..., :, None] - b[..., None, :] + i_pre[..., None, :], -jnp.inf)
    m_intra = jnp.max(d_mat, axis=-1)
    b_last = b[..., -1]
    g_end = b_last[..., None] - b + i_pre
    g_end_max = jnp.max(g_end, axis=-1)
    qk = jnp.einsum('bhncd,bhnsd->bhncs', q, k)

    def step(carry, xs):
        c_s, n_s, m_s = carry
        q_c, k_c, v_c, qk_c, d_c, mi_c, b_c, bl_c, ge_c, gm_c = xs
        m_t = jnp.maximum(b_c + m_s[..., None], mi_c)
        inter = jnp.exp(b_c + m_s[..., None] - m_t)
        p = jnp.exp(d_c - m_t[..., None]) * qk_c
        num = (inter[..., None] * jnp.einsum('bhcd,bhde->bhce', q_c, c_s)
               + jnp.einsum('bhcs,bhse->bhce', p, v_c))
        den = inter * jnp.einsum('bhcd,bhd->bhc', q_c, n_s) + jnp.sum(p, axis=-1)
        h = num / jnp.maximum(jnp.abs(den), jnp.exp(-m_t))[..., None]
        m_new = jnp.maximum(bl_c + m_s, gm_c)
        keep = jnp.exp(bl_c + m_s - m_new)
        kw = k_c * jnp.exp(ge_c - m_new[..., None])[..., None]
        c_s = keep[..., None, None] * c_s + jnp.einsum('bhsd,bhse->bhde', kw, v_c)
        n_s = keep[..., None] * n_s + jnp.sum(kw, axis=-2)
        return (c_s, n_s, m_new), h

    xs = tuple(jnp.moveaxis(t, 2, 0) for t in
               (q, k, v, qk, d_mat, m_intra, b, b_last, g_end, g_end_max))
    carry0 = (jnp.zeros((B, ML_HEADS, ML_DK, ML_DV), jnp.float32),
              jnp.zeros((B, ML_HEADS, ML_DK), jnp.float32),
              jnp.zeros((B, ML_HEADS), jnp.float32))
    _, h = lax.scan(step, carry0, xs)
    h = from_chunks(jnp.moveaxis(h, 0, 2))
    h = rms_norm(h, norm_w.reshape(ML_HEADS, ML_DV))
    o_gate = jax.nn.sigmoid(proj[..., OFF_ML_O:OFF_ML_I]).reshape(B, S, ML_HEADS, ML_DV)
    return (h * o_gate).reshape(B, S, ML_V)


def moe(h, w_router, b_router, w_gate_up, b_gate_up, w_down, b_down):
    B, S, D = h.shape
    T = B * S
    xf = h.reshape(T, D)
    logits = (xf @ w_router + b_router).astype(jnp.float32)
    top_logit, top_idx = lax.top_k(logits, TOP_K)
    top_w = jax.nn.softmax(top_logit, axis=-1)
    n_assign = T * TOP_K
    n_blocks = n_assign // MOE_BLOCK + N_EXPERTS
    e_flat = top_idx.reshape(-1)
    order = jnp.argsort(e_flat)
    sorted_e = e_flat[order]
    sorted_tok = (order // TOP_K).astype(jnp.int32)
    sorted_w = top_w.reshape(-1)[order]
    counts = jnp.bincount(e_flat, length=N_EXPERTS)
    padded = (counts + MOE_BLOCK - 1) // MOE_BLOCK * MOE_BLOCK
    start = jnp.cumsum(counts) - counts
    pad_end = jnp.cumsum(padded)
    pad_start = pad_end - padded
    dest = pad_start[sorted_e] + jnp.arange(n_assign, dtype=jnp.int32) - start[sorted_e]
    slot_tok = jnp.full((n_blocks * MOE_BLOCK,), T, jnp.int32).at[dest].set(sorted_tok)
    slot_w = jnp.zeros((n_blocks * MOE_BLOCK,), jnp.float32).at[dest].set(sorted_w)
    block_e = jnp.minimum(
        jnp.searchsorted(pad_end, jnp.arange(n_blocks, dtype=pad_end.dtype) * MOE_BLOCK, side='right'),
        N_EXPERTS - 1)
    x_pad = jnp.concatenate([xf, jnp.zeros((1, D), xf.dtype)], axis=0)

    def expert_block(args):
        tok, e = args
        xb = x_pad[tok]
        gu = xb @ w_gate_up[e] + b_gate_up[e]
        gate = jnp.minimum(gu[:, :D_FF], SWIGLU_LIMIT)
        up = jnp.clip(gu[:, D_FF:], -SWIGLU_LIMIT, SWIGLU_LIMIT)
        act = (up + 1.0) * gate * jax.nn.sigmoid(SWIGLU_ALPHA * gate)
        return act @ w_down[e] + b_down[e]

    y = lax.map(expert_block, (slot_tok.reshape(n_blocks, MOE_BLOCK), block_e))
    y = y.reshape(-1, D) * slot_w[:, None].astype(y.dtype)
    out = jnp.zeros((T + 1, D), y.dtype).at[slot_tok].add(y)[:T]
    return out.reshape(B, S, D).astype(h.dtype)


def setup_inputs(seed: int = 0) -> dict:
    key = jax.random.key(seed)
    ks = jax.random.split(key, 26)
    f32 = jnp.float32

    def nrm(k, shape, scale):
        return scale * jax.random.normal(k, shape, f32)

    x = nrm(ks[0], (BATCH, SEQ, D_MODEL), 1.0)
    c = nrm(ks[1], (BATCH, D_MODEL), 1.0)
    w_ada = nrm(ks[2], (DEPTH, D_MODEL, 6 * D_MODEL), D_MODEL ** -0.5)
    b_ada = nrm(ks[3], (DEPTH, 6 * D_MODEL), 0.02)
    norm_mix = 1.0 + nrm(ks[4], (DEPTH, D_MODEL), 0.02)
    w_in = nrm(ks[5], (DEPTH, D_MODEL, D_IN), D_MODEL ** -0.5)
    dn_conv = nrm(ks[6], (DEPTH, CONV_W, DN_CONV_CH), CONV_W ** -0.5)
    dn_a_log = jnp.log(jax.random.uniform(ks[7], (DEPTH, DN_HEADS), f32, 1.0, 16.0))
    dt = jnp.exp(jax.random.uniform(ks[8], (DEPTH, DN_HEADS), f32, math.log(1e-3), math.log(1e-1)))
    dn_dt_bias = dt + jnp.log(-jnp.expm1(-dt))
    dn_norm = 1.0 + nrm(ks[9], (DEPTH, DN_DV), 0.02)
    ml_conv = nrm(ks[10], (DEPTH, CONV_W, 2 * ML_QK), CONV_W ** -0.5)
    ml_i_bias = nrm(ks[11], (DEPTH, ML_HEADS), 0.1)
    ml_f_bias = jnp.linspace(3.0, 6.0, ML_HEADS, dtype=f32) + nrm(ks[12], (DEPTH, ML_HEADS), 0.1)
    ml_norm = 1.0 + nrm(ks[13], (DEPTH, ML_V), 0.02)
    w_out = nrm(ks[14], (DEPTH, D_MIX, D_MODEL), D_MIX ** -0.5)
    norm_ffn = 1.0 + nrm(ks[15], (DEPTH, D_MODEL), 0.02)
    w_router = nrm(ks[16], (DEPTH, D_MODEL, N_EXPERTS), D_MODEL ** -0.5)
    b_router = nrm(ks[17], (DEPTH, N_EXPERTS), 0.01)
    w_gate_up = nrm(ks[18], (DEPTH, N_EXPERTS, D_MODEL, 2 * D_FF), D_MODEL ** -0.5)
    b_gate_up = nrm(ks[19], (DEPTH, N_EXPERTS, 2 * D_FF), 0.02)
    w_down = nrm(ks[20], (DEPTH, N_EXPERTS, D_FF, D_MODEL), D_FF ** -0.5)
    b_down = nrm(ks[21], (DEPTH, N_EXPERTS, D_MODEL), 0.02)
    w_ada_final = nrm(ks[22], (D_MODEL, 2 * D_MODEL), D_MODEL ** -0.5)
    b_ada_final = nrm(ks[23], (2 * D_MODEL,), 0.02)
    norm_final = 1.0 + nrm(ks[24], (D_MODEL,), 0.02)
    return {'x': x, 'c': c, 'w_ada': w_ada, 'b_ada': b_ada, 'norm_mix': norm_mix,
            'w_in': w_in, 'dn_conv': dn_conv, 'dn_a_log': dn_a_log, 'dn_dt_bias': dn_dt_bias,
            'dn_norm': dn_norm, 'ml_conv': ml_conv, 'ml_i_bias': ml_i_bias,
            'ml_f_bias': ml_f_bias, 'ml_norm': ml_norm, 'w_out': w_out, 'norm_ffn': norm_ffn,
            'w_router': w_router, 'b_router': b_router, 'w_gate_up': w_gate_up,
            'b_gate_up': b_gate_up, 'w_down': w_down, 'b_down': b_down,
            'w_ada_final': w_ada_final, 'b_ada_final': b_ada_final, 'norm_final': norm_final}


def reference(x, c, w_ada, b_ada, norm_mix, w_in, dn_conv, dn_a_log, dn_dt_bias, dn_norm,
              ml_conv, ml_i_bias, ml_f_bias, ml_norm, w_out, norm_ffn, w_router, b_router,
              w_gate_up, b_gate_up, w_down, b_down, w_ada_final, b_ada_final, norm_final):
    cond = jax.nn.silu(c)
    for l in range(DEPTH):
        mod = (cond @ w_ada[l] + b_ada[l])[:, None, :]
        sh1, sc1, g1, sh2, sc2, g2 = jnp.split(mod, 6, axis=-1)
        h = modulate(rms_norm(x, norm_mix[l]), sh1, sc1)
        proj = (h @ w_in[l]).astype(jnp.float32)
        y_a = gated_deltanet(proj, dn_conv[l], dn_a_log[l], dn_dt_bias[l], dn_norm[l])
        y_b = mlstm(proj, ml_conv[l], ml_i_bias[l], ml_f_bias[l], ml_norm[l])
        mix = jnp.concatenate([y_a, y_b], axis=-1).astype(x.dtype) @ w_out[l]
        x = x + g1 * mix
        h = modulate(rms_norm(x, norm_ffn[l]), sh2, sc2)
        x = x + g2 * moe(h, w_router[l], b_router[l], w_gate_up[l], b_gate_up[l],
                         w_down[l], b_down[l])
    sh_f, sc_f = jnp.split((cond @ w_ada_final + b_ada_final)[:, None, :], 2, axis=-1)
    return modulate(rms_norm(x, norm_final), sh_f, sc_f)
```

```python
import numpy as np
import concourse.bass as bass
import concourse.mybir as mybir
from concourse.bass_utils import run_bass_kernel_spmd
from contextlib import ExitStack

dt = mybir.dt
F32, BF16, I32, U8 = dt.float32, dt.bfloat16, dt.int32, dt.uint8
ALU = mybir.AluOpType
AF = mybir.ActivationFunctionType
AX = mybir.AxisListType

D = 1024
SEQ = 4096
NB = 4
KC = 8
CH = 64
EPS = 1e-6
D_IN = 3600
NEG = -60000.0

DEBUG = {}
DEBUG_ON = False
SIM_NBLK = 0
SUB = 0
PSUB = 0
EXP = {}
SIM_NE = 0


def _dsize(d):
    if d == F32 or d == I32:
        return 4
    if d == BF16:
        return 2
    if d == U8:
        return 1
    raise ValueError(str(d))


def _rect(ap):
    a = ap.ap
    ds = _dsize(ap.dtype)
    name = ap.tensor.name
    if str(ap.space) == "DRAM":
        ext = sum((c - 1) * abs(s) for s, c in a)
        return (name, 0, 1, ap.offset * ds, (ap.offset + ext + 1) * ds)
    pstep, pcnt = a[0]
    if pstep == 0:
        pstep = 1 << 40
    p0 = ap.offset // pstep
    fo = ap.offset % pstep
    ext = sum((c - 1) * abs(s) for s, c in a[1:])
    b0, b1 = fo * ds, (fo + ext + 1) * ds
    p1 = p0 + pcnt
    if str(ap.space) == "PSUM":
        b0 = b0 // 2048 * 2048
        b1 = (b1 + 2047) // 2048 * 2048
        p0 = 0
        p1 = 128
    return (name, p0, p1, b0, b1)


class Op:
    __slots__ = ("eng", "fn", "deps", "dma_key", "dma_val", "count", "signal", "waits", "clock", "dma_snap", "idx")

    def __init__(self, eng, fn):
        self.eng = eng
        self.fn = fn
        self.deps = {}
        self.dma_key = None
        self.dma_val = 0
        self.count = 0
        self.signal = False
        self.waits = []
        self.clock = None
        self.dma_snap = None


ENGS = ("pe", "act", "dve", "pool", "sp")


class Prog:
    def __init__(self):
        self.ops = []
        self.eng_ops = {e: [] for e in ENGS}
        self.hist = {}
        self.dma_count = {}
        self.dma_waiters = {}
        self.whole = set()

    def _dep(self, op, prod):
        if prod is op:
            return
        if prod.eng == "pe" and op.eng == "pe" and prod.dma_key is None and op.dma_key is None:
            return
        op.deps[id(prod)] = prod

    def _access(self, op, ap, write):
        name, p0, p1, b0, b1 = _rect(ap)
        if name in self.whole:
            p0, p1, b0, b1 = 0, 1 << 30, 0, 1 << 60
        lst = self.hist.setdefault(name, [])
        keep = []
        ch = ("d", op.dma_key) if op.dma_key is not None else ("e", op.eng)
        for ent in lst:
            q0, q1, c0, c1, prod, w, pch = ent
            ov = (q0 < p1 and p0 < q1 and c0 < b1 and b0 < c1)
            if ov and (write or w):
                self._dep(op, prod)
            elif ov and name == "psum" and prod.eng != op.eng:
                self._dep(op, prod)
            if write and q0 >= p0 and q1 <= p1 and c0 >= b0 and c1 <= b1:
                continue
            if (not write) and (not w) and pch == ch and q0 == p0 and q1 == p1 and c0 == b0 and c1 == b1:
                continue
            keep.append(ent)
        keep.append((p0, p1, b0, b1, op, write, ch))
        self.hist[name] = keep

    def add(self, eng, fn, outs, ins, dma_key=None):
        op = Op(eng, fn)
        op.idx = len(self.ops)
        if dma_key is not None:
            op.dma_key = dma_key
            for w in self.dma_waiters.get(dma_key, []):
                self._dep(op, w)
            self.dma_waiters[dma_key] = []
        for ap in ins:
            self._access(op, ap, False)
        for ap in outs:
            self._access(op, ap, True)
        snap = {}
        for prod in op.deps.values():
            if prod.dma_key is not None:
                k = prod.dma_key
                snap[k] = 16 * self.dma_count[k]
                self.dma_waiters.setdefault(k, []).append(op)
        op.dma_snap = snap
        if dma_key is not None:
            self.dma_count[dma_key] = self.dma_count.get(dma_key, 0) + 1
            op.dma_val = 16 * self.dma_count[dma_key]
        self.ops.append(op)
        self.eng_ops[eng].append(op)
        return op

    def finalize(self):
        for op in self.ops:
            for prod in op.deps.values():
                if prod.dma_key is None:
                    prod.signal = True
        cnt = {e: 0 for e in ENGS}
        for op in self.ops:
            if op.dma_key is None and op.signal:
                cnt[op.eng] += 1
                op.count = cnt[op.eng]
        last_clock = {e: {} for e in ENGS}
        for op in self.ops:
            base = dict(last_clock[op.eng])
            needs = []
            for prod in op.deps.values():
                if prod.dma_key is not None:
                    needs.append((("d", prod.dma_key), op.dma_snap[prod.dma_key], prod))
                else:
                    needs.append((("e", prod.eng), prod.count, prod))
            needs.sort(key=lambda t: -t[2].idx)
            waits = []
            for ch, val, prod in needs:
                if base.get(ch, 0) >= val:
                    continue
                waits.append((ch, val))
                for k, v in prod.clock.items():
                    if base.get(k, 0) < v:
                        base[k] = v
                base[ch] = max(base.get(ch, 0), val)
            wd = {}
            for ch, val in waits:
                wd[ch] = max(wd.get(ch, 0), val)
            op.waits = list(wd.items())
            last_clock[op.eng] = base
            oc = dict(base)
            if op.dma_key is not None:
                oc[("d", op.dma_key)] = max(oc.get(("d", op.dma_key), 0), op.dma_val)
            elif op.signal:
                oc[("e", op.eng)] = max(oc.get(("e", op.eng), 0), op.count)
            op.clock = oc
        return cnt

    def emit(self, nc, stack):
        cnt = self.finalize()
        sems = {}
        for e in ENGS:
            sems[("e", e)] = stack.enter_context(nc.semaphore("s_" + e))
        for k in self.dma_count:
            sems[("d", k)] = stack.enter_context(nc.semaphore("d_" + str(k)))
        self.sems = sems
        block = stack.enter_context(nc.Block())
        eng_ops = self.eng_ops

        def run(engine, ops):
            for op in ops:
                for ch, val in op.waits:
                    engine.wait_ge(sems[ch], val)
                ins = op.fn(engine)
                if op.dma_key is not None:
                    ins.then_inc(sems[("d", op.dma_key)], 16)
                elif op.signal:
                    ins.then_inc(sems[("e", op.eng)], 1)

        @block.tensor
        def _(e):
            run(e, eng_ops["pe"])

        @block.scalar
        def _(e):
            run(e, eng_ops["act"])

        @block.vector
        def _(e):
            run(e, eng_ops["dve"])

        @block.gpsimd
        def _(e):
            run(e, eng_ops["pool"])

        @block.sync
        def _(e):
            run(e, eng_ops["sp"])
        return cnt


class K:
    def __init__(self, nc, stack):
        self.nc = nc
        self.P = Prog()
        self.arena_bytes = 206 * 1024
        self.arena = stack.enter_context(nc.sbuf_tensor("arena", [128, self.arena_bytes], U8))
        self.psum = stack.enter_context(nc.psum_tensor("psum", [128, 4096], F32))
        self.top = 0
        self.marks = []
        self.ps_small = 0
        self.ps_wide = 0
        self.dve_toggle = 0

    def alloc(self, shape, dtype):
        n = 1
        for s in shape:
            n *= s
        nbytes = n * _dsize(dtype)
        off = (self.top + 63) // 64 * 64
        assert off + nbytes <= self.arena_bytes, ("SBUF arena overflow", off, nbytes)
        self.top = off + nbytes
        v = self.arena[:, off:off + nbytes].bitcast(dtype)
        if len(shape) == 2:
            v = v.rearrange("p (a b) -> p a b", a=shape[0])
        elif len(shape) == 3:
            v = v.rearrange("p (a b c) -> p a b c", a=shape[0], b=shape[1])
        return v

    def push(self):
        self.marks.append(self.top)

    def pop(self):
        self.top = self.marks.pop()

    def ps(self, cols=128):
        b = self.ps_wide % 8
        self.ps_wide += 1
        return self.psum[:, b * 512:b * 512 + cols]

    def mm(self, out, lhsT, rhs, start=True, stop=True):
        return self.P.add("pe", lambda e: e.matmul(out, lhsT, rhs, start=start, stop=stop), [out], [lhsT, rhs])

    def tr(self, out, in_, ident):
        if EXP.get("notr", 1):
            return self.P.add("pe", lambda e: e.matmul(out, in_, ident, start=True, stop=True), [out], [in_, ident])
        return self.P.add("pe", lambda e: e.transpose(out, in_, ident), [out], [in_, ident])

    def act(self, out, in_, func, bias=None, scale=None, eng="act"):
        ins = [in_]
        kw = {}
        if bias is not None:
            kw["bias"] = bias
            if not isinstance(bias, (int, float)):
                ins.append(bias)
        if scale is not None:
            kw["scale"] = scale
            if not isinstance(scale, (int, float)):
                ins.append(scale)
        return self.P.add(eng, lambda e: e.activation(out, in_, func, **kw), [out], ins)

    def tt(self, out, in0, in1, op, eng="dve"):
        return self.P.add(eng, lambda e: e.tensor_tensor(out, in0, in1, op), [out], [in0, in1])

    def ts(self, out, in0, s1, s2, op0, op1=None, eng="dve"):
        ins = [in0]
        for s in (s1, s2):
            if s is not None and not isinstance(s, (int, float)):
                ins.append(s)
        if op1 is None:
            return self.P.add(eng, lambda e: e.tensor_scalar(out, in0, s1, None, op0), [out], ins)
        return self.P.add(eng, lambda e: e.tensor_scalar(out, in0, s1, s2, op0, op1), [out], ins)

    def stt(self, out, in0, scalar, in1, op0, op1, eng="dve"):
        ins = [in0, in1]
        if not isinstance(scalar, (int, float)):
            ins.append(scalar)
        return self.P.add(eng, lambda e: e.scalar_tensor_tensor(out, in0, scalar, in1, op0, op1), [out], ins)

    def copy(self, out, in_, eng="dve"):
        if eng == "act":
            return self.P.add("act", lambda e: e.copy(out, in_), [out], [in_])
        return self.P.add(eng, lambda e: e.tensor_copy(out, in_), [out], [in_])

    def recip(self, out, in_):
        return self.P.add("dve", lambda e: e.reciprocal(out, in_), [out], [in_])

    def rmax(self, out, in_):
        return self.P.add("dve", lambda e: e.reduce_max(out, in_, AX.X), [out], [in_])

    def rsum(self, out, in_):
        return self.P.add("dve", lambda e: e.reduce_sum(out, in_, AX.X), [out], [in_])

    def memset(self, ap, val, eng="dve"):
        return self.P.add(eng, lambda e: e.memset(ap, val), [ap], [])

    def dma(self, out, in_, key, eng="sp"):
        return self.P.add(eng, lambda e: e.dma_start(out=out, in_=in_), [out], [in_], dma_key=key)


C_ID, C_ONES, C_TRI, C_MNEG, C_STRICT, C_MLOW, C_EPS, C_N = 0, 128, 256, 320, 384, 448, 512, 520


def make_consts():
    c = np.zeros((128, C_N), np.float32)
    c[:, C_ID:C_ID + 128] = np.eye(128, dtype=np.float32)
    c[:, C_ONES:C_ONES + 128] = 1.0
    s = np.arange(64)[:, None]
    cc = np.arange(64)[None, :]
    c[:64, C_TRI:C_TRI + 64] = (s <= cc)
    c[:64, C_MNEG:C_MNEG + 64] = np.where(s <= cc, 0.0, NEG)
    c[:64, C_STRICT:C_STRICT + 64] = (s < cc)
    c[:64, C_MLOW:C_MLOW + 64] = np.where(cc <= s, 0.0, NEG)
    c[:, C_EPS] = EPS
    c[:, C_EPS + 1] = 1.0
    return c


INPUT_SPECS = [
    ("xT", [128, 8, SEQ], F32), ("xo", [128, 8, 2048], F32), ("cT", [128, 8], F32),
    ("wada", [128, 8, 8192], F32), ("bada", [128, 64], F32), ("norms", [128, 24], F32),
    ("win", [128, 8, D_IN], F32), ("dnconv", [128, 12, 4], F32), ("mlconv", [128, 4, 4], F32),
    ("hp", [128, 16], F32), ("dnnorm", [128, 1], F32), ("mlnorm", [128, 4], F32),
    ("wout", [128, 8, 1024], F32), ("wr", [128, 8, 32], F32), ("br", [128, 32], F32),
    ("wgu", [32, 1024, 2048], F32), ("bgu", [128, 32, 16], F32),
    ("wd", [32, 1024, 1024], F32), ("bd", [32, 1024], F32),
    ("consts", [128, C_N], F32), ("gidx", [128, 8], I32),
]


def build(stage=99, dbg=False):
    nc = bass.Bass("TRN2", target_bir_lowering=False)
    T = {}
    for name, shape, dtp in INPUT_SPECS:
        T[name] = nc.dram_tensor(name, shape, dtp, kind="ExternalInput").ap()
    outT = nc.dram_tensor("outT", [128, 8, 2048], F32, kind="ExternalOutput").ap()
    ybuf = nc.dram_tensor("ybuf", [8 * 128 * 2, 2048], BF16, kind="Internal").ap()
    T["x1buf"] = nc.dram_tensor("x1buf", [128, 8, 2048], F32, kind="Internal").ap()
    dbg_t = None
    if dbg:
        dbg_t = nc.dram_tensor("dbg", [128, 8192], F32, kind="ExternalOutput").ap()
    stack = ExitStack()
    with stack:
        k = K(nc, stack)
        P = k.P
        dbg_off = [0]

        def dump(name, ap):
            if not dbg:
                return
            p, n = ap.shape[0], ap.shape[1]
            DEBUG[name] = (dbg_off[0], p, n)
            if ap.dtype != F32:
                k.push()
                tmpd = k.alloc([n], F32)
                k.copy(tmpd[0:p], ap)
                k.dma(dbg_t[0:p, dbg_off[0]:dbg_off[0] + n], tmpd[0:p], "dbg")
                k.pop()
                dbg_off[0] += n
                return
            k.dma(dbg_t[0:p, dbg_off[0]:dbg_off[0] + n], ap, "dbg")
            dbg_off[0] += n

        consts = k.alloc([C_N], F32)
        k.dma(consts, T["consts"], "c0")
        ident = consts[:, C_ID:C_ID + 128]
        ones = consts[:, C_ONES:C_ONES + 128]
        epsc = consts[:, C_EPS:C_EPS + 1]
        ones_bf = k.alloc([128], BF16)
        k.copy(ones_bf, ones)
        ident_bf = k.alloc([128], BF16)
        k.copy(ident_bf, ident)
        norms = k.alloc([24], F32)
        k.dma(norms, T["norms"], "c0")
        mod = k.alloc([64], F32)
        modw = k.alloc([24], F32)

        k.push()
        cT = k.alloc([8], F32)
        k.dma(cT, T["cT"], "c0")
        bada = k.alloc([64], F32)
        k.dma(bada, T["bada"], "c0")
        cond = k.alloc([8], F32)
        k.act(cond, cT, AF.Silu)
        wbuf = [k.alloc([8, 1024], F32) for _ in range(2)]
        modps = k.ps(128)
        for blk in range(8):
            wb = wbuf[blk % 2]
            k.dma(wb, T["wada"][:, :, blk * 1024:(blk + 1) * 1024], "wa%d" % (blk % 2))
            for jj in range(8):
                j = blk * 8 + jj
                for kc in range(8):
                    k.mm(modps[:, j:j + 1], wb[:, kc, jj * 128:(jj + 1) * 128], cond[:, kc:kc + 1],
                         start=(kc == 0), stop=(kc == 7))
        k.tt(mod, modps[:, 0:64], bada, ALU.add)
        k.stt(modw[:, 0:8], mod[:, 8:16], 1.0, norms[:, 0:8], ALU.add, ALU.mult)
        k.stt(modw[:, 8:16], mod[:, 32:40], 1.0, norms[:, 8:16], ALU.add, ALU.mult)
        k.stt(modw[:, 16:24], mod[:, 56:64], 1.0, norms[:, 16:24], ALU.add, ALU.mult)
        k.pop()
        SH1, G1, SH2, G2, SHF = mod[:, 0:8], mod[:, 16:24], mod[:, 24:32], mod[:, 40:48], mod[:, 48:56]
        W1, W2, WF = modw[:, 0:8], modw[:, 8:16], modw[:, 16:24]
        dump("mod", mod)
        dump("modw", modw)

        def norm_block(xb, n, wv, shv, hT_out):
            k.push()
            sq = k.alloc([8, n], BF16)
            k.act(sq, xb, AF.Square)
            ss = k.ps(n)
            for kc in range(8):
                k.mm(ss, ones_bf, sq[:, kc, :], start=(kc == 0), stop=(kc == 7))
            rstd = k.alloc([n], F32)
            k.act(rstd, ss, AF.Sqrt, bias=epsc, scale=1.0 / D)
            k.recip(rstd, rstd)
            tmp = k.alloc([2, n], F32)
            for kc in range(8):
                k.tt(tmp[:, kc % 2, :], xb[:, kc, :], rstd, ALU.mult)
                k.act(hT_out[:, kc, :], tmp[:, kc % 2, :], AF.Identity, bias=shv[:, kc:kc + 1], scale=wv[:, kc:kc + 1])
            k.pop()

        if stage >= 1:
            mixer_phase(k, T, dict(ident=ident, ones=ones, ones_bf=ones_bf, ident_bf=ident_bf, consts=consts,
                                   epsc=epsc, W1=W1, SH1=SH1), norm_block, ybuf, dump, stage)

        if stage >= 4:
            post_phase(k, nc, T, dict(ident=ident, ones=ones, ones_bf=ones_bf, consts=consts, epsc=epsc,
                                      W2=W2, SH2=SH2, G1=G1, G2=G2, WF=WF, SHF=SHF), norm_block, ybuf, outT, dump)

        fin_ins = [outT]
        if dbg:
            fin_ins.append(dbg_t)
        P.add("sp", lambda e: e.nop(), [], fin_ins)
        cnt = P.emit(nc, stack)
    return nc, cnt


FM = ([("dq%d" % h, h * 128) for h in range(4)] + [("dk%d" % h, 512 + h * 128) for h in range(4)] +
      [("dv%d" % h, 1024 + h * 128) for h in range(4)] + [("mq%d" % i, 2056 + i * 128) for i in range(2)] +
      [("mk%d" % i, 2312 + i * 128) for i in range(2)] + [("dz%d" % h, 1536 + h * 128) for h in range(4)] +
      [("mo%d" % h, 3080 + h * 128) for h in range(4)])
FMI = {n: i for i, (n, _) in enumerate(FM)}
NCONV = 16
BLK = 256


def mixer_phase(k, T, C, norm_block, ybuf, dump, stage):
    ident, ones, ones_bf, consts, epsc = C["ident"], C["ones"], C["ones_bf"], C["consts"], C["epsc"]
    onec = consts[:, C_EPS + 1:C_EPS + 2]
    tri = consts[0:64, C_TRI:C_TRI + 64]
    mneg = consts[0:64, C_MNEG:C_MNEG + 64]
    strict = consts[0:64, C_STRICT:C_STRICT + 64]
    mlow = consts[0:64, C_MLOW:C_MLOW + 64]
    id64 = ident[0:64, 0:64]
    k.push()
    win_bf = k.alloc([8, D_IN], BF16)
    k.push()
    stg = [k.alloc([8, 450], F32) for _ in range(2)]
    for i in range(8):
        s = stg[i % 2]
        k.dma(s, T["win"][:, :, i * 450:(i + 1) * 450], "ws%d" % (i % 2))
        k.copy(win_bf[:, :, i * 450:(i + 1) * 450], s, eng=("act" if i % 2 else "dve"))
    k.pop()
    dnconv = k.alloc([12, 4], F32)
    k.dma(dnconv, T["dnconv"], "c1")
    mlconv = k.alloc([4, 4], F32)
    k.dma(mlconv, T["mlconv"], "c1")
    hp = k.alloc([16], F32)
    k.dma(hp, T["hp"], "c1")
    dnnorm = k.alloc([1], F32)
    k.dma(dnnorm, T["dnnorm"], "c1")
    mlnorm = k.alloc([4], F32)
    k.dma(mlnorm, T["mlnorm"], "c1")
    nega = k.alloc([4], F32)
    k.act(nega, hp[:, 0:4], AF.Exp)
    k.ts(nega, nega, -1.0, None, ALU.mult)

    xb = k.alloc([8, BLK], F32)
    hT = k.alloc([8, BLK], BF16)
    pb = k.alloc([len(FM), 3 + BLK], F32)
    cv = k.alloc([NCONV, BLK], F32)
    cvb = k.alloc([NCONV, BLK], BF16)
    gz = k.alloc([8, BLK], F32)
    ytile = k.alloc([8, BLK], BF16)
    k.memset(pb[:, :, 0:3], 0.0)

    def A(shape, dtp):
        return k.alloc(shape, dtp)
    GD = []
    for h in range(4):
        d = dict(S=A([128], F32), Sb=A([128], BF16), grep=A([128], F32), brep=A([64], F32), Dm=A([64], F32),
                 E=A([64], F32), Bs=A([64], F32), M=A([64], F32), MT=A([64], F32), U=A([64], F32),
                 QK=A([64], BF16), Pa=A([64], F32), PTa=A([64], F32), Pb=A([64], F32), PTb=A([64], F32),
                 kb=A([128], F32), kdec=A([128], BF16), vb=A([128], F32), w=A([128], F32),
                 kcT=A([64], BF16), eGb=A([64], F32), qdT=A([64], BF16), vnew=A([128], BF16),
                 osq=A([64], BF16), rn=A([64], F32), y=A([64], F32))
        k.memset(d["S"], 0.0)
        k.memset(d["Sb"], 0.0)
        GD.append(d)
    ML = []
    for h in range(4):
        d = dict(CN=A([256], F32), CNb=A([256], BF16), ms=A([1], F32), lrep=A([128], F32), nrep=A([128], F32),
                 bb=A([64], F32), nab=A([64], F32), amax=A([1], F32), dcs=A([64], F32), cmx=A([1], F32),
                 mto=A([1], F32), mrep=A([128], F32), tmpm=A([64], F32), D2=A([64], F32), pT=A([64], BF16),
                 inter=A([64], F32), qiT=A([64], BF16), flo=A([64], F32), dn=A([64], F32), hT=A([64], F32),
                 hsq=A([64], BF16), rn=A([64], F32), mx=A([1], F32), keep=A([1], F32), ew=A([1], F32),
                 kw=A([128], BF16))
        k.memset(d["CN"], 0.0)
        k.memset(d["CNb"], 0.0)
        k.memset(d["ms"], 0.0)
        k.memset(d["kw"], 0.0)
        ML.append(d)
    gtm = A([16], F32)
    gt = dict(beta=A([4], F32), x=A([4], F32), ax=A([4], F32), e=A([4], F32), r=A([4], F32), g=A([4], F32),
              G=A([4], F32), Gl=A([4], F32), ebg=A([4], F32), edl=A([4], F32),
              ip=A([4], F32), xf=A([4], F32), lf=A([4], F32), b=A([4], F32), na=A([4], F32))
    vaug = A([4, 256], BF16)
    k.memset(vaug[:, :, 128:256], 1.0)
    ktm = [A([128], F32) for _ in range(2)]
    kz = [A([BLK], BF16) for _ in range(4)]
    for h in range(4):
        k.memset(kz[h], 0.0)

    def softplus(out, x, P_=64):
        k.stt(gt["ax"][0:P_], x, -1.0, x, ALU.mult, ALU.max)
        k.act(gt["e"][0:P_], gt["ax"][0:P_], AF.Exp, scale=-1.0)
        k.act(gt["e"][0:P_], gt["e"][0:P_], AF.Ln, bias=onec[0:P_], scale=1.0)
        k.ts(gt["r"][0:P_], x, 0.0, None, ALU.max)
        k.tt(out, gt["r"][0:P_], gt["e"][0:P_], ALU.add)

    nblk = SEQ // BLK if stage >= 3 else 1
    if SIM_NBLK:
        nblk = SIM_NBLK
    for blk in range(nblk):
        t0 = blk * BLK
        if blk > 0:
            k.copy(pb[:, :, 0:3], pb[:, :, BLK:BLK + 3], eng="pool")
        k.dma(xb, T["xT"][:, :, t0:t0 + BLK], "xb")
        norm_block(xb, BLK, C["W1"], C["SH1"], hT)
        if blk == 0:
            dump("hT0", hT[:, 0, :])
        for ci, (nm, c0) in enumerate(FM):
            ps = k.ps(BLK)
            for kc in range(8):
                k.mm(ps, win_bf[:, kc, c0:c0 + 128], hT[:, kc, :], start=(kc == 0), stop=(kc == 7))
            k.copy(pb[:, ci, 3:3 + BLK], ps, eng=("act" if ci % 2 else "dve"))
        if blk == 0:
            dump("pq0", pb[:, FMI["dq0"], 3:3 + BLK])
            dump("mo3", pb[:, FMI["mo3"], 3:3 + BLK])
        if stage < 2:
            continue
        for ci in range(NCONV):
            wv = dnconv[:, ci, :] if ci < 12 else mlconv[:, ci - 12, :]
            k.ts(cv[:, ci, :], pb[:, ci, 0:BLK], wv[:, 0:1], None, ALU.mult, eng="pool")
            for j in range(1, 4):
                k.stt(cv[:, ci, :], pb[:, ci, j:j + BLK], wv[:, j:j + 1], cv[:, ci, :], ALU.mult, ALU.add)
            k.act(cv[:, ci, :], cv[:, ci, :], AF.Silu)
        if SUB == 1:
            dump("cvA", cv[:, 0, :])
            return
        for ci in range(8):
            sq = cvb[:, ci, :]
            k.act(sq, cv[:, ci, :], AF.Square)
            ss = k.ps(BLK)
            k.mm(ss, ones_bf, sq)
            rn = gz[:, 0, :]
            k.act(rn, ss, AF.Sqrt, bias=epsc, scale=1.0)
            k.recip(rn, rn)
            if ci < 4:
                k.stt(cv[:, ci, :], cv[:, ci, :], 128.0 ** -0.5, rn, ALU.mult, ALU.mult)
            else:
                k.tt(cv[:, ci, :], cv[:, ci, :], rn, ALU.mult)
        for ci in (12, 13):
            k.ts(cv[:, ci, :], cv[:, ci, :], 0.125, None, ALU.mult, eng="pool")
        k.copy(cvb, cv, eng="pool")
        for h in range(4):
            p0_ = (h % 2) * 64
            k.copy(kz[h][p0_:p0_ + 64, :], cv[p0_:p0_ + 64, 14 + h // 2, :], eng="pool")
        for h in range(4):
            k.act(gz[:, h, :], pb[:, FMI["dz%d" % h], 3:3 + BLK], AF.Silu)
            k.act(gz[:, 4 + h, :], pb[:, FMI["mo%d" % h], 3:3 + BLK], AF.Sigmoid)
        if blk == 0:
            dump("k0n", cv[:, 4, :])
            dump("mq0", cv[:, 12, :])

        if SUB == 2:
            return
        for j in range(EXP.get("nj", BLK // CH)):
            c0 = j * CH
            cs = slice(c0, c0 + CH)
            gps = k.ps(128)
            for kc in range(8):
                k.mm(gps[0:64, 0:8], hT[:, kc, cs], win_bf[:, kc, 2048:2056], start=(kc == 0), stop=(kc == 7))
            for kc in range(8):
                k.mm(gps[0:64, 8:16], hT[:, kc, cs], win_bf[:, kc, 3592:3600], start=(kc == 0), stop=(kc == 7))
            k.copy(gtm[0:64], gps[0:64, 0:16])
            vps = k.ps(512)
            for kc in range(8):
                k.mm(vps[0:64, :], hT[:, kc, cs], win_bf[:, kc, 2568:3080], start=(kc == 0), stop=(kc == 7))
            k.copy(vaug[0:64, :, 0:128], vps[0:64, :].rearrange("p (h e) -> p h e", h=4), eng="act")
            g = gt
            k.act(g["beta"][0:64], gtm[0:64, 0:4], AF.Sigmoid)
            k.tt(g["x"][0:64], gtm[0:64, 4:8], hp[0:64, 4:8], ALU.add)
            softplus(g["g"][0:64], g["x"][0:64])
            k.tt(g["g"][0:64], g["g"][0:64], nega[0:64], ALU.mult)
            Gps = k.ps(128)
            k.mm(Gps[0:64, 0:4], tri, g["g"][0:64])
            k.mm(Gps[0:64, 4:8], ones[0:64, 0:64], g["g"][0:64])
            k.copy(g["G"][0:64], Gps[0:64, 0:4])
            k.tt(g["edl"][0:64], Gps[0:64, 4:8], g["G"][0:64], ALU.subtract)
            k.act(g["edl"][0:64], g["edl"][0:64], AF.Exp)
            k.act(g["ebg"][0:64], g["G"][0:64], AF.Exp)
            k.tt(g["ebg"][0:64], g["ebg"][0:64], g["beta"][0:64], ALU.mult)
            k.tt(g["ip"][0:64], gtm[0:64, 8:12], hp[0:64, 8:12], ALU.add)
            k.tt(g["xf"][0:64], gtm[0:64, 12:16], hp[0:64, 12:16], ALU.add)
            k.ts(g["xf"][0:64], g["xf"][0:64], -1.0, None, ALU.mult)
            softplus(g["lf"][0:64], g["xf"][0:64])
            k.ts(g["lf"][0:64], g["lf"][0:64], -1.0, None, ALU.mult)
            bps = k.ps(128)
            k.mm(bps[0:64, 0:4], tri, g["lf"][0:64])
            k.copy(g["b"][0:64], bps[0:64, 0:4])
            k.tt(g["na"][0:64], g["ip"][0:64], g["b"][0:64], ALU.subtract)
            if blk == 0 and j == 0:
                dump("G0", g["G"][0:64])
                dump("b0", g["b"][0:64])
                dump("gtm", gtm[0:64])
                dump("hp", hp[0:64])
                dump("ip_a", g["ip"][0:64])
                dump("lf_a", g["lf"][0:64])

            if SUB == 3:
                return
            for pr in range(2):
                tkp = k.ps(128)
                k.tr(tkp[0:64, :], cv[:, 14 + pr, cs], ident)
                k.copy(ktm[pr][0:64], tkp[0:64, :], eng="act")
            def gdn_head(h):
                d = GD[h]
                bank = k.psum[:, h * 512:(h + 1) * 512]
                RA, RB, RC = bank[:, 0:128], bank[:, 128:256], bank[:, 256:512]
                kT, kTb = cv[:, 4 + h, cs], cvb[:, 4 + h, cs]
                qT, qTb = cv[:, h, cs], cvb[:, h, cs]
                vT = cv[:, 8 + h, cs]
                k.copy(d["grep"][0:64], g["g"][0:64, h:h + 1].to_broadcast([64, 128]), eng="pool")
                yield
                k.copy(d["brep"][0:64], g["beta"][0:64, h:h + 1].to_broadcast([64, 64]), eng="pool")
                yield
                Gb = RA
                k.mm(Gb[:, 0:64], d["grep"][0:64], tri)
                yield
                k.mm(Gb[0:64, 64:128], d["brep"][0:64], id64)
                yield
                k.act(d["eGb"], Gb[:, 0:64], AF.Exp)
                yield
                sc = RB
                k.mm(sc[0:64, 0:64], kTb, kTb)
                yield
                k.mm(sc[0:64, 64:128], kTb, qTb)
                yield
                k.stt(d["Dm"][0:64], Gb[0:64, 0:64], g["G"][0:64, h:h + 1], mneg, ALU.subtract, ALU.add)
                yield
                k.act(d["E"][0:64], d["Dm"][0:64], AF.Exp)
                yield
                k.tt(d["Bs"][0:64], Gb[0:64, 64:128], strict, ALU.mult)
                yield
                k.tt(d["M"][0:64], sc[0:64, 0:64], d["E"][0:64], ALU.mult)
                yield
                k.tt(d["M"][0:64], d["M"][0:64], d["Bs"][0:64], ALU.mult)
                yield
                k.tt(d["QK"][0:64], sc[0:64, 64:128], d["E"][0:64], ALU.mult)
                yield
                tp = RC
                k.tr(tp[0:64, 0:64], d["M"][0:64], id64)
                yield
                k.copy(d["MT"][0:64], tp[0:64, 0:64], eng=("dve" if EXP.get("mtdve") else "act"))
                yield
                k.tt(d["U"][0:64], id64, d["M"][0:64], ALU.subtract, eng="pool")
                yield
                Pc, PTc = d["M"], d["MT"]
                bufs = [(d["Pa"], d["PTa"]), (d["Pb"], d["PTb"])]
                for lev in range(1, EXP.get("nlev", 5) + 1):
                    Pn, PTn = bufs[lev % 2]
                    pp = RC
                    if EXP.get("v") == 1:
                        k.mm(pp[0:64, 0:64], Pc[0:64], Pc[0:64])
                        yield
                        k.copy(PTn[0:64], pp[0:64, 0:64])
                        yield
                        return
                    if EXP.get("v") == 2:
                        k.mm(pp[0:64, 0:64], id64, Pc[0:64])
                        yield
                        k.copy(PTn[0:64], pp[0:64, 0:64])
                        yield
                        return
                    if EXP.get("v") == 4:
                        k.mm(pp[0:64, 0:64], Pc[0:64], PTc[0:64])
                        yield
                        k.copy(PTn[0:64], pp[0:64, 0:64])
                        yield
                        return
                    if EXP.get("v") == 6:
                        k.mm(pp[0:64, 0:64], Pc[0:64], PTc[0:64])
                        yield
                        k.mm(pp[0:64, 64:128], PTc[0:64], Pc[0:64])
                        yield
                        k.copy(PTn[0:64], pp[0:64, 0:64])
                        yield
                        k.copy(Pn[0:64], pp[0:64, 64:128])
                        yield
                        return
                    if EXP.get("v") == 7:
                        k.mm(pp[0:64, 0:64], PTc[0:64], Pc[0:64])
                        yield
                        k.copy(PTn[0:64], pp[0:64, 0:64])
                        yield
                        return
                    if EXP.get("v") == 3:
                        k.mm(pp[0:64, 0:64], Pc[0:64], id64)
                        yield
                        k.copy(PTn[0:64], pp[0:64, 0:64])
                        yield
                        return
                    k.mm(pp[0:64, 0:64], Pc[0:64], PTc[0:64])
                    yield
                    if lev < 5:
                        k.mm(pp[0:64, 64:128], PTc[0:64], Pc[0:64])
                        yield
                    k.copy(PTn[0:64], pp[0:64, 0:64], eng="act")
                    yield
                    if lev < 5:
                        k.copy(Pn[0:64], pp[0:64, 64:128])
                        yield
                    if EXP.get("noup"):
                        Pc, PTc = Pn, PTn
                        continue
                    up = RA
                    k.mm(up[0:64, 0:64], PTn[0:64], d["U"][0:64])
                    yield
                    k.tt(d["U"][0:64], d["U"][0:64], up[0:64, 0:64], ALU.add)
                    yield
                    Pc, PTc = Pn, PTn
                tk = RC
                k.tr(tk[0:64, 0:128], kT, ident)
                yield
                k.tr(tk[0:64, 128:256], vT, ident)
                yield
                k.ts(d["kb"][0:64], tk[0:64, 0:128], g["ebg"][0:64, h:h + 1], None, ALU.mult)
                yield
                k.ts(d["kdec"][0:64], tk[0:64, 0:128], g["edl"][0:64, h:h + 1], None, ALU.mult)
                yield
                k.ts(d["vb"][0:64], tk[0:64, 128:256], g["beta"][0:64, h:h + 1], None, ALU.mult)
                yield
                wk = RC
                k.mm(wk[0:64, 0:128], d["U"][0:64], d["vb"][0:64])
                yield
                k.mm(wk[:, 128:192], d["kb"][0:64], d["U"][0:64])
                yield
                k.copy(d["w"][0:64], wk[0:64, 0:128], eng="act")
                yield
                k.copy(d["kcT"], wk[:, 128:192])
                yield
                k.tt(d["qdT"], qT, d["eGb"], ALU.mult, eng="pool")
                yield
                p1 = RA
                k.mm(p1[0:64, :], d["kcT"], d["Sb"])
                yield
                k.tt(d["vnew"][0:64], d["w"][0:64], p1[0:64, :], ALU.subtract)
                yield
                op_ = RB
                k.mm(op_[:, 0:64], d["Sb"], d["qdT"], start=True, stop=False)
                yield
                k.mm(op_[:, 0:64], d["vnew"][0:64], d["QK"][0:64], start=False, stop=True)
                yield
                dS = RC[:, 0:128]
                k.mm(dS, d["kdec"][0:64], d["vnew"][0:64])
                yield
                k.stt(d["S"], d["S"], d["eGb"][:, 63:64], dS, ALU.mult, ALU.add)
                yield
                k.copy(d["Sb"], d["S"], eng="act")
                yield
                k.act(d["osq"], op_[:, 0:64], AF.Square)
                yield
                ss = RA
                k.mm(ss[:, 0:64], ones_bf, d["osq"])
                yield
                k.act(d["rn"], ss[:, 0:64], AF.Sqrt, bias=epsc, scale=1.0 / 128)
                yield
                k.recip(d["rn"], d["rn"])
                yield
                k.stt(d["y"], op_[:, 0:64], dnnorm[:, 0:1], d["rn"], ALU.mult, ALU.mult)
                yield
                k.tt(ytile[:, h, cs], d["y"], gz[:, h, cs], ALU.mult, eng="pool")
                yield
                if blk == 0 and j == 0 and h == 0:
                    dump("M0", d["M"][0:64])
                    dump("U0", d["U"][0:64])
                    dump("y0", d["y"])

            def ml_head(h):
                d = ML[h]
                bank = k.psum[:, (4 + h) * 512:(5 + h) * 512]
                RA, RB, RC = bank[:, 0:128], bank[:, 128:256], bank[:, 256:512]
                pr, p0 = h // 2, (h % 2) * 64
                psl = slice(p0, p0 + 64)
                qT = cv[psl, 12 + pr, cs]
                qTb, kTb = cvb[psl, 12 + pr, cs], cvb[psl, 14 + pr, cs]
                k.copy(d["lrep"][0:64], g["lf"][0:64, h:h + 1].to_broadcast([64, 128]), eng="pool")
                yield
                k.copy(d["nrep"][0:64], g["na"][0:64, h:h + 1].to_broadcast([64, 128]), eng="pool")
                yield
                bp = RA
                k.mm(bp[:, 0:64], d["lrep"][0:64], tri)
                yield
                k.mm(bp[:, 64:128], d["nrep"][0:64], id64)
                yield
                k.copy(d["bb"], bp[:, 0:64], eng="act")
                yield
                k.copy(d["nab"], bp[:, 64:128])
                yield
                k.rmax(d["amax"], d["nab"])
                yield
                k.tt(d["dcs"][0:64], d["nab"][0:64], mlow, ALU.add, eng="pool")
                yield
                k.rmax(d["cmx"][0:64], d["dcs"][0:64])
                yield
                k.tt(d["mto"][0:64], d["cmx"][0:64], d["ms"][0:64], ALU.max)
                yield
                k.copy(d["mrep"][0:64], d["mto"][0:64, 0:1].to_broadcast([64, 128]), eng="pool")
                yield
                mp = RB
                k.mm(mp[:, 0:64], d["mrep"][0:64], id64)
                yield
                sc = RC
                k.mm(sc[0:64, 0:64], kz[h][:, cs], cvb[:, 12 + pr, cs])
                yield
                k.ts(d["tmpm"][0:64], mneg, g["na"][0:64, h:h + 1], None, ALU.add, eng="pool")
                yield
                k.stt(d["D2"][0:64], mp[0:64, 0:64], -1.0, d["tmpm"][0:64], ALU.mult, ALU.add)
                yield
                k.act(d["D2"][0:64], d["D2"][0:64], AF.Exp)
                yield
                k.tt(d["pT"][0:64], sc[0:64, 0:64], d["D2"][0:64], ALU.mult)
                yield
                k.act(d["inter"], mp[:, 0:64], AF.Exp, bias=d["ms"], scale=-1.0)
                yield
                k.tt(d["qiT"], cv[:, 12 + pr, cs], d["inter"], ALU.mult, eng="pool")
                yield
                k.tt(d["flo"], d["bb"], mp[:, 0:64], ALU.add)
                yield
                k.act(d["flo"], d["flo"], AF.Exp, scale=-1.0)
                yield
                nd = RC[:, 0:128]
                k.mm(nd[:, 0:64], d["CNb"][:, 0:128], d["qiT"], start=True, stop=False)
                yield
                k.mm(nd[:, 0:64], vaug[0:64, h, 0:128], d["pT"][0:64], start=False, stop=True)
                yield
                k.mm(nd[:, 64:128], d["CNb"][:, 128:256], d["qiT"], start=True, stop=False)
                yield
                k.mm(nd[:, 64:128], vaug[0:64, h, 128:256], d["pT"][0:64], start=False, stop=True)
                yield
                k.ts(d["dn"], nd[:, 64:128], -1.0, None, ALU.mult)
                yield
                k.tt(d["dn"], d["dn"], nd[:, 64:128], ALU.max)
                yield
                k.tt(d["dn"], d["dn"], d["flo"], ALU.max)
                yield
                k.recip(d["dn"], d["dn"])
                yield
                k.tt(d["hT"], nd[:, 0:64], d["dn"], ALU.mult)
                yield
                k.act(d["hsq"], d["hT"], AF.Square)
                yield
                ss = RA
                k.mm(ss[:, 0:64], ones_bf, d["hsq"])
                yield
                k.act(d["rn"], ss[:, 0:64], AF.Sqrt, bias=epsc, scale=1.0 / 128)
                yield
                k.recip(d["rn"], d["rn"])
                yield
                k.stt(d["hT"], d["hT"], mlnorm[:, h:h + 1], d["rn"], ALU.mult, ALU.mult)
                yield
                k.tt(ytile[:, 4 + h, cs], d["hT"], gz[:, 4 + h, cs], ALU.mult, eng="pool")
                yield
                k.tt(d["mx"], d["ms"], d["amax"], ALU.max)
                yield
                k.tt(d["keep"], d["ms"], d["mx"], ALU.subtract, eng="pool")
                yield
                k.act(d["keep"], d["keep"], AF.Exp)
                yield
                k.tt(d["ew"][0:64], g["na"][0:64, h:h + 1], d["mx"][0:64], ALU.subtract)
                yield
                k.act(d["ew"][0:64], d["ew"][0:64], AF.Exp)
                yield
                k.ts(d["kw"][0:64, p0:p0 + 64], ktm[pr][0:64, p0:p0 + 64], d["ew"][0:64, 0:1], None, ALU.mult)
                yield
                dc = RC
                k.mm(dc, d["kw"][0:64], vaug[0:64, h, :])
                yield
                k.stt(d["CN"], d["CN"], d["keep"][:, 0:1], dc, ALU.mult, ALU.add)
                yield
                k.copy(d["CNb"], d["CN"], eng="act")
                yield
                k.tt(d["ms"], d["bb"][:, 63:64], d["mx"], ALU.add, eng="pool")
                yield
                if blk == 0 and j == 0 and h == 0:
                    dump("mh0", d["hT"])
                    dump("nab", d["nab"])
                    dump("lrep", d["lrep"][0:64])
                    dump("nrep", d["nrep"][0:64])
                    dump("lf", g["lf"][0:64])
                    dump("na", g["na"][0:64])
                    dump("ip", g["ip"][0:64])
                    dump("bb", d["bb"])
                    dump("inter", d["inter"])
                    dump("flo", d["flo"])
                    dump("dn", d["dn"])
                    dump("pT", d["pT"][0:64])
                    dump("qiT", d["qiT"][0:64])
                    dump("rnm", d["rn"])

            gens = [gdn_head(h) for h in range(4)] + [ml_head(h) for h in range(4)]
            while gens:
                for g_ in list(gens):
                    try:
                        next(g_)
                    except StopIteration:
                        gens.remove(g_)
        if SUB == 5:
            return
        for hh in range(0 if EXP.get("noyb") else 8):
            half, off = t0 // 2048, t0 % 2048
            r0 = (half * 8 + hh) * 128
            k.dma(ybuf[r0:r0 + 128, off:off + BLK], ytile[:, hh, :], "yb")
        if blk == 0:
            k.push()
            yd = k.alloc([BLK], F32)
            for hh in (0, 4):
                k.copy(yd, ytile[:, hh, :])
                dump("yt%d" % hh, yd)
            k.pop()
    k.pop()


def _kc(a):
    a = np.asarray(a, np.float32)
    return np.ascontiguousarray(a.reshape((8, 128) + a.shape[1:]).swapaxes(0, 1))


def prep_inputs(inp):
    f = np.float32
    x = np.asarray(inp["x"], f)
    consts = make_consts()
    w_all = np.concatenate([np.asarray(inp["w_ada"][0], f), np.asarray(inp["w_ada_final"], f)], axis=1)
    b_all = np.concatenate([np.asarray(inp["b_ada"][0], f), np.asarray(inp["b_ada_final"], f)])
    shared = {
        "wada": _kc(w_all),
        "bada": np.ascontiguousarray(b_all.reshape(64, 128).T),
        "norms": np.ascontiguousarray(np.concatenate(
            [np.asarray(inp[n], f).reshape(8, 128).T for n in ("norm_mix", "norm_ffn", "norm_final")], axis=1)),
        "win": _kc(inp["w_in"][0]),
        "dnconv": np.ascontiguousarray(np.asarray(inp["dn_conv"][0], f).reshape(4, 12, 128).transpose(2, 1, 0)),
        "mlconv": np.ascontiguousarray(np.asarray(inp["ml_conv"][0], f).reshape(4, 4, 128).transpose(2, 1, 0)),
        "hp": np.ascontiguousarray(np.broadcast_to(np.concatenate(
            [np.asarray(inp[n][0], f) for n in ("dn_a_log", "dn_dt_bias", "ml_i_bias", "ml_f_bias")])[None, :], (128, 16))),
        "dnnorm": np.ascontiguousarray(np.asarray(inp["dn_norm"][0], f).reshape(128, 1)),
        "mlnorm": np.ascontiguousarray(np.asarray(inp["ml_norm"][0], f).reshape(4, 128).T),
        "wout": _kc(inp["w_out"][0]),
        "wr": _kc(inp["w_router"][0]),
        "br": np.ascontiguousarray(np.broadcast_to(np.asarray(inp["b_router"][0], f)[None, :], (128, 32))),
        "wgu": np.ascontiguousarray(np.asarray(inp["w_gate_up"][0], f)),
        "bgu": np.ascontiguousarray(np.asarray(inp["b_gate_up"][0], f).reshape(32, 16, 128).transpose(2, 0, 1)),
        "wd": np.ascontiguousarray(np.asarray(inp["w_down"][0], f)),
        "bd": np.ascontiguousarray(np.asarray(inp["b_down"][0], f)),
        "consts": consts,
    }
    maps = []
    for c in range(8):
        b, hf = c // 2, c % 2
        xT = _kc(x[b].T)
        m = dict(shared)
        m["xT"] = xT
        m["xo"] = np.ascontiguousarray(xT[:, :, hf * 2048:(hf + 1) * 2048])
        m["cT"] = np.ascontiguousarray(np.asarray(inp["c"], f)[b].reshape(8, 128).T)
        p = np.arange(128, dtype=np.int32)[:, None]
        h = np.arange(8, dtype=np.int32)[None, :]
        m["gidx"] = np.ascontiguousarray(((hf * 8 + h) * 128 + p).astype(np.int32))
        maps.append(m)
    return maps


def post_phase(k, nc, T, C, norm_block, ybuf, outT, dump):
    ident, epsc = C["ident"], C["epsc"]
    P = k.P
    P.whole.add("ybuf")
    NT = 2048
    TB = 512
    k.push()
    gidx = k.alloc([8], I32)
    k.dma(gidx, T["gidx"], "c2")
    h2T = k.alloc([8, NT], BF16)
    for hh in range(8):
        P.add("pool", (lambda hh: lambda e: e.indirect_dma_start(
            out=h2T[:, hh, :], out_offset=None, in_=ybuf[:, :],
            in_offset=bass.IndirectOffsetOnAxis(ap=gidx[:, hh:hh + 1], axis=0)))(hh),
            [h2T[:, hh, :]], [ybuf, gidx[:, hh:hh + 1]], dma_key="yg")
    if PSUB == 1:
        k.push()
        yd_ = k.alloc([512], F32)
        k.copy(yd_, h2T[:, 0, 0:512])
        dump("yg0", yd_)
        k.pop()
        return
    GT = k.alloc([NT], F32)
    wr = k.alloc([8, 32], F32)
    k.dma(wr, T["wr"], "c2")
    br = k.alloc([32], F32)
    k.dma(br, T["br"], "c2")
    bd = k.alloc([1024], F32)
    k.dma(bd[0:32], T["bd"], "c2")
    bgu = k.alloc([32, 16], F32)
    k.dma(bgu, T["bgu"], "c2")
    stg = [k.alloc([2048], F32) for _ in range(2)]
    sti = [0]

    def load_cast(dst_bf, src, n):
        s_ = stg[sti[0] % 2]
        key = "st%d" % (sti[0] % 2)
        sti[0] += 1
        k.dma(s_[:, 0:n], src, key)
        k.copy(dst_bf, s_[:, 0:n], eng="pool")

    k.push()
    wout_bf = k.alloc([8, 1024], BF16)
    for yc in range(8):
        load_cast(wout_bf[:, yc, :], T["wout"][:, yc, :], 1024)
    xob = k.alloc([8, TB], F32)
    x1b = k.alloc([8, TB], F32)
    h2f = k.alloc([8, TB], F32)
    lg = k.alloc([32], F32)
    mx8 = k.alloc([8], F32)
    nm1 = k.alloc([1], F32)
    msk = k.alloc([32], F32)
    ex = k.alloc([32], F32)
    den = k.alloc([1], F32)
    for tb in range(NT // TB):
        ts_ = slice(tb * TB, (tb + 1) * TB)
        k.dma(xob, T["xo"][:, :, ts_], "xo")
        for dc in range(8):
            ps = k.ps(TB)
            for yc in range(8):
                k.mm(ps, wout_bf[:, yc, dc * 128:(dc + 1) * 128], h2T[:, yc, ts_], start=(yc == 0), stop=(yc == 7))
            k.stt(x1b[:, dc, :], ps, C["G1"][:, dc:dc + 1], xob[:, dc, :], ALU.mult, ALU.add)
        k.dma(T["x1buf"][:, :, ts_], x1b, "x1w")
        if tb == 0:
            dump("x1", x1b[:, 0, :])
        norm_block(x1b, TB, C["W2"], C["SH2"], h2f)
        k.copy(h2T[:, :, ts_], h2f, eng="pool")
        for tt_ in range(TB // 128):
            tsl = slice(tt_ * 128, (tt_ + 1) * 128)
            lp = k.ps(128)
            for kc in range(8):
                k.mm(lp[:, 0:32], h2f[:, kc, tsl], wr[:, kc, :], start=(kc == 0), stop=(kc == 7))
            k.tt(lg, lp[:, 0:32], br, ALU.add)
            P.add("dve", lambda e: e.max(mx8, lg), [mx8], [lg])
            k.ts(msk, lg, mx8[:, 3:4], None, ALU.is_ge)
            k.ts(nm1, mx8[:, 0:1], -1.0, None, ALU.mult)
            k.act(ex, lg, AF.Exp, bias=nm1, scale=1.0)
            k.tt(ex, ex, msk, ALU.mult)
            k.rsum(den, ex)
            k.recip(den, den)
            k.ts(ex, ex, den[:, 0:1], None, ALU.mult)
            tp = k.ps(128)
            k.tr(tp[0:32, 0:128], ex, ident)
            g0 = tb * TB + tt_ * 128
            k.copy(GT[0:32, g0:g0 + 128], tp[0:32, 0:128], eng="act")
            if tb == 0 and tt_ == 0:
                dump("lg", lg)
                dump("gate", ex)
    k.pop()
    if PSUB == 2:
        return

    acc = k.alloc([8, NT], F32)
    k.push()
    wgu_bf = k.alloc([8, 2048], BF16)
    wd_bf = k.alloc([8, 1024], BF16)
    sel = k.alloc([128], F32)
    gbe = k.alloc([TB], F32)
    actT = k.alloc([8, TB], BF16)
    tg = [k.alloc([TB], F32) for _ in range(2)]
    tsg = [k.alloc([TB], F32) for _ in range(2)]
    tu = [k.alloc([TB], F32) for _ in range(2)]
    for tb in range(NT // TB):
        ts_ = slice(tb * TB, (tb + 1) * TB)
        for dc in range(8):
            ps = k.ps(TB)
            k.mm(ps, bd[0:32, dc * 128:(dc + 1) * 128], GT[0:32, ts_])
            k.copy(acc[:, dc, ts_], ps, eng=("act" if dc % 2 else "dve"))
    if EXP.get("m") == 0:
        return
    for kc in range(8):
        load_cast(wgu_bf[:, kc, :], T["wgu"][0, kc * 128:(kc + 1) * 128, :], 2048)
    if EXP.get("m") == 1:
        return
    NE = SIM_NE or 32
    it = 0
    for e in range(NE):
        for fc in range(8):
            load_cast(wd_bf[:, fc, :], T["wd"][e, fc * 128:(fc + 1) * 128, :], 1024)
        k.copy(sel[0:32], ident[0:32, e:e + 1].to_broadcast([32, 128]))
        for tb in range(NT // TB):
            ts_ = slice(tb * TB, (tb + 1) * TB)
            gp = k.ps(TB)
            k.mm(gp, sel[0:32], GT[0:32, ts_])
            k.copy(gbe, gp, eng="act")
            if EXP.get("m") == 2:
                return
            for fc in range(8):
                b = it % 2
                it += 1
                gps = k.ps(TB)
                for kc in range(8):
                    k.mm(gps, wgu_bf[:, kc, fc * 128:(fc + 1) * 128], h2T[:, kc, ts_], start=(kc == 0), stop=(kc == 7))
                ups = k.ps(TB)
                for kc in range(8):
                    k.mm(ups, wgu_bf[:, kc, 1024 + fc * 128:1024 + (fc + 1) * 128], h2T[:, kc, ts_],
                         start=(kc == 0), stop=(kc == 7))
                k.ts(tg[b], gps, bgu[:, e, fc:fc + 1], 7.0, ALU.add, ALU.min)
                k.act(tsg[b], tg[b], AF.Sigmoid, scale=1.702)
                k.ts(tu[b], ups, bgu[:, e, 8 + fc:9 + fc], 7.0, ALU.add, ALU.min)
                if EXP.get("m") == 3:
                    return
                k.ts(tu[b], tu[b], -7.0, 1.0, ALU.max, ALU.add, eng="pool")
                k.tt(tg[b], tg[b], tsg[b], ALU.mult)
                k.tt(tu[b], tu[b], gbe, ALU.mult, eng="pool")
                k.tt(actT[:, fc, :], tg[b], tu[b], ALU.mult, eng="pool")
                if EXP.get("m") == 4:
                    return
            if EXP.get("m") == 5:
                return
            if e + 1 < NE and tb == NT // TB - 1:
                for kc in range(8):
                    load_cast(wgu_bf[:, kc, :], T["wgu"][e + 1, kc * 128:(kc + 1) * 128, :], 2048)
            for dc in range(8):
                ps = k.ps(TB)
                for fc in range(8):
                    k.mm(ps, wd_bf[:, fc, dc * 128:(dc + 1) * 128], actT[:, fc, :], start=(fc == 0), stop=(fc == 7))
                k.tt(acc[:, dc, ts_], acc[:, dc, ts_], ps, ALU.add)
            if EXP.get("m") == 6:
                return
            if EXP.get("m") == 7 and tb == 1:
                return
            if EXP.get("m") == 8 and tb == 3:
                dump("acc", acc[:, 0, 0:512])
                return
    dump("acc", acc[:, 0, 0:512])
    k.pop()
    if PSUB == 3:
        return

    k.push()
    x1b = k.alloc([8, TB], F32)
    of = k.alloc([8, TB], F32)
    for tb in range(NT // TB):
        ts_ = slice(tb * TB, (tb + 1) * TB)
        k.dma(x1b, T["x1buf"][:, :, ts_], "x1r")
        for dc in range(8):
            k.stt(x1b[:, dc, :], acc[:, dc, ts_], C["G2"][:, dc:dc + 1], x1b[:, dc, :], ALU.mult, ALU.add)
        norm_block(x1b, TB, C["WF"], C["SHF"], of)
        k.dma(outT[:, :, ts_], of, "out")
    k.pop()
    k.pop()


_CACHE = {}


def kernel(**inputs):
    if "nc" not in _CACHE:
        _CACHE["nc"] = build(stage=99, dbg=False)[0]
    nc = _CACHE["nc"]
    maps = prep_inputs(inputs)
    res = run_bass_kernel_spmd(nc, maps, core_ids=list(range(8)))
    out = np.zeros((NB, SEQ, D), np.float32)
    for c in range(8):
        b, hf = c // 2, c % 2
        o = np.asarray(res.results[c]["outT"])
        out[b, hf * 2048:(hf + 1) * 2048, :] = o.transpose(2, 1, 0).reshape(2048, D)
    return out
```

```python
import numpy as np
import concourse.bass as bass
import concourse.mybir as mybir
from concourse.bass_utils import run_bass_kernel_spmd
from contextlib import ExitStack

dt = mybir.dt
F32, BF16, I32, U8 = dt.float32, dt.bfloat16, dt.int32, dt.uint8
ALU = mybir.AluOpType
AF = mybir.ActivationFunctionType
AX = mybir.AxisListType

D = 1024
SEQ = 4096
NB = 4
KC = 8
CH = 64
EPS = 1e-6
D_IN = 3600
NEG = -60000.0

DEBUG = {}
DEBUG_ON = False
SIM_NBLK = 0
SUB = 0
PSUB = 0
EXP = {}
SIM_NE = 0


def _dsize(d):
    if d == F32 or d == I32:
        return 4
    if d == BF16:
        return 2
    if d == U8:
        return 1
    raise ValueError(str(d))


def _rect(ap):
    a = ap.ap
    ds = _dsize(ap.dtype)
    name = ap.tensor.name
    if str(ap.space) == "DRAM":
        ext = sum((c - 1) * abs(s) for s, c in a)
        return (name, 0, 1, ap.offset * ds, (ap.offset + ext + 1) * ds)
    pstep, pcnt = a[0]
    if pstep == 0:
        pstep = 1 << 40
    p0 = ap.offset // pstep
    fo = ap.offset % pstep
    ext = sum((c - 1) * abs(s) for s, c in a[1:])
    b0, b1 = fo * ds, (fo + ext + 1) * ds
    p1 = p0 + pcnt
    if str(ap.space) == "PSUM":
        b0 = b0 // 2048 * 2048
        b1 = (b1 + 2047) // 2048 * 2048
        p0 = 0
        p1 = 128
    return (name, p0, p1, b0, b1)


class Op:
    __slots__ = ("eng", "fn", "deps", "dma_key", "dma_val", "count", "signal", "waits", "clock", "dma_snap", "idx")

    def __init__(self, eng, fn):
        self.eng = eng
        self.fn = fn
        self.deps = {}
        self.dma_key = None
        self.dma_val = 0
        self.count = 0
        self.signal = False
        self.waits = []
        self.clock = None
        self.dma_snap = None


ENGS = ("pe", "act", "dve", "pool", "sp")


class Prog:
    def __init__(self):
        self.ops = []
        self.eng_ops = {e: [] for e in ENGS}
        self.hist = {}
        self.dma_count = {}
        self.dma_waiters = {}
        self.whole = set()

    def _dep(self, op, prod):
        if prod is op:
            return
        if prod.eng == "pe" and op.eng == "pe" and prod.dma_key is None and op.dma_key is None:
            return
        op.deps[id(prod)] = prod

    def _access(self, op, ap, write):
        name, p0, p1, b0, b1 = _rect(ap)
        if name in self.whole:
            p0, p1, b0, b1 = 0, 1 << 30, 0, 1 << 60
        lst = self.hist.setdefault(name, [])
        keep = []
        ch = ("d", op.dma_key) if op.dma_key is not None else ("e", op.eng)
        for ent in lst:
            q0, q1, c0, c1, prod, w, pch = ent
            ov = (q0 < p1 and p0 < q1 and c0 < b1 and b0 < c1)
            if ov and (write or w):
                self._dep(op, prod)
            elif ov and name == "psum" and prod.eng != op.eng:
                self._dep(op, prod)
            if write and q0 >= p0 and q1 <= p1 and c0 >= b0 and c1 <= b1:
                continue
            if (not write) and (not w) and pch == ch and q0 == p0 and q1 == p1 and c0 == b0 and c1 == b1:
                continue
            keep.append(ent)
        keep.append((p0, p1, b0, b1, op, write, ch))
        self.hist[name] = keep

    def add(self, eng, fn, outs, ins, dma_key=None):
        op = Op(eng, fn)
        op.idx = len(self.ops)
        if dma_key is not None:
            op.dma_key = dma_key
            for w in self.dma_waiters.get(dma_key, []):
                self._dep(op, w)
            self.dma_waiters[dma_key] = []
        for ap in ins:
            self._access(op, ap, False)
        for ap in outs:
            self._access(op, ap, True)
        snap = {}
        for prod in op.deps.values():
            if prod.dma_key is not None:
                k = prod.dma_key
                snap[k] = 16 * self.dma_count[k]
                self.dma_waiters.setdefault(k, []).append(op)
        op.dma_snap = snap
        if dma_key is not None:
            self.dma_count[dma_key] = self.dma_count.get(dma_key, 0) + 1
            op.dma_val = 16 * self.dma_count[dma_key]
        self.ops.append(op)
        self.eng_ops[eng].append(op)
        return op

    def finalize(self):
        for op in self.ops:
            for prod in op.deps.values():
                if prod.dma_key is None:
                    prod.signal = True
        cnt = {e: 0 for e in ENGS}
        for op in self.ops:
            if op.dma_key is None and op.signal:
                cnt[op.eng] += 1
                op.count = cnt[op.eng]
        last_clock = {e: {} for e in ENGS}
        for op in self.ops:
            base = dict(last_clock[op.eng])
            needs = []
            for prod in op.deps.values():
                if prod.dma_key is not None:
                    needs.append((("d", prod.dma_key), op.dma_snap[prod.dma_key], prod))
                else:
                    needs.append((("e", prod.eng), prod.count, prod))
            needs.sort(key=lambda t: -t[2].idx)
            waits = []
            for ch, val, prod in needs:
                if base.get(ch, 0) >= val:
                    continue
                waits.append((ch, val))
                for k, v in prod.clock.items():
                    if base.get(k, 0) < v:
                        base[k] = v
                base[ch] = max(base.get(ch, 0), val)
            wd = {}
            for ch, val in waits:
                wd[ch] = max(wd.get(ch, 0), val)
            op.waits = list(wd.items())
            last_clock[op.eng] = base
            oc = dict(base)
            if op.dma_key is not None:
                oc[("d", op.dma_key)] = max(oc.get(("d", op.dma_key), 0), op.dma_val)
            elif op.signal:
                oc[("e", op.eng)] = max(oc.get(("e", op.eng), 0), op.count)
            op.clock = oc
        return cnt

    def emit(self, nc, stack):
        cnt = self.finalize()
        sems = {}
        for e in ENGS:
            sems[("e", e)] = stack.enter_context(nc.semaphore("s_" + e))
        for k in self.dma_count:
            sems[("d", k)] = stack.enter_context(nc.semaphore("d_" + str(k)))
        self.sems = sems
        block = stack.enter_context(nc.Block())
        eng_ops = self.eng_ops

        def run(engine, ops):
            for op in ops:
                for ch, val in op.waits:
                    engine.wait_ge(sems[ch], val)
                ins = op.fn(engine)
                if op.dma_key is not None:
                    ins.then_inc(sems[("d", op.dma_key)], 16)
                elif op.signal:
                    ins.then_inc(sems[("e", op.eng)], 1)

        @block.tensor
        def _(e):
            run(e, eng_ops["pe"])

        @block.scalar
        def _(e):
            run(e, eng_ops["act"])

        @block.vector
        def _(e):
            run(e, eng_ops["dve"])

        @block.gpsimd
        def _(e):
            run(e, eng_ops["pool"])

        @block.sync
        def _(e):
            run(e, eng_ops["sp"])
        return cnt


class K:
    def __init__(self, nc, stack):
        self.nc = nc
        self.P = Prog()
        self.arena_bytes = 206 * 1024
        self.arena = stack.enter_context(nc.sbuf_tensor("arena", [128, self.arena_bytes], U8))
        self.psum = stack.enter_context(nc.psum_tensor("psum", [128, 4096], F32))
        self.top = 0
        self.marks = []
        self.ps_small = 0
        self.ps_wide = 0
        self.dve_toggle = 0

    def alloc(self, shape, dtype):
        n = 1
        for s in shape:
            n *= s
        nbytes = n * _dsize(dtype)
        off = (self.top + 63) // 64 * 64
        assert off + nbytes <= self.arena_bytes, ("SBUF arena overflow", off, nbytes)
        self.top = off + nbytes
        v = self.arena[:, off:off + nbytes].bitcast(dtype)
        if len(shape) == 2:
            v = v.rearrange("p (a b) -> p a b", a=shape[0])
        elif len(shape) == 3:
            v = v.rearrange("p (a b c) -> p a b c", a=shape[0], b=shape[1])
        return v

    def push(self):
        self.marks.append(self.top)

    def pop(self):
        self.top = self.marks.pop()

    def ps(self, cols=128):
        b = self.ps_wide % 8
        self.ps_wide += 1
        return self.psum[:, b * 512:b * 512 + cols]

    def mm(self, out, lhsT, rhs, start=True, stop=True):
        return self.P.add("pe", lambda e: e.matmul(out, lhsT, rhs, start=start, stop=stop), [out], [lhsT, rhs])

    def tr(self, out, in_, ident):
        if EXP.get("notr", 1):
            return self.P.add("pe", lambda e: e.matmul(out, in_, ident, start=True, stop=True), [out], [in_, ident])
        return self.P.add("pe", lambda e: e.transpose(out, in_, ident), [out], [in_, ident])

    def act(self, out, in_, func, bias=None, scale=None, eng="act"):
        ins = [in_]
        kw = {}
        if bias is not None:
            kw["bias"] = bias
            if not isinstance(bias, (int, float)):
                ins.append(bias)
        if scale is not None:
            kw["scale"] = scale
            if not isinstance(scale, (int, float)):
                ins.append(scale)
        return self.P.add(eng, lambda e: e.activation(out, in_, func, **kw), [out], ins)

    def tt(self, out, in0, in1, op, eng="dve"):
        return self.P.add(eng, lambda e: e.tensor_tensor(out, in0, in1, op), [out], [in0, in1])

    def ts(self, out, in0, s1, s2, op0, op1=None, eng="dve"):
        ins = [in0]
        for s in (s1, s2):
            if s is not None and not isinstance(s, (int, float)):
                ins.append(s)
        if op1 is None:
            return self.P.add(eng, lambda e: e.tensor_scalar(out, in0, s1, None, op0), [out], ins)
        return self.P.add(eng, lambda e: e.tensor_scalar(out, in0, s1, s2, op0, op1), [out], ins)

    def stt(self, out, in0, scalar, in1, op0, op1, eng="dve"):
        ins = [in0, in1]
        if not isinstance(scalar, (int, float)):
            ins.append(scalar)
        return self.P.add(eng, lambda e: e.scalar_tensor_tensor(out, in0, scalar, in1, op0, op1), [out], ins)

    def copy(self, out, in_, eng="dve"):
        if eng == "act":
            return self.P.add("act", lambda e: e.copy(out, in_), [out], [in_])
        return self.P.add(eng, lambda e: e.tensor_copy(out, in_), [out], [in_])

    def recip(self, out, in_):
        return self.P.add("dve", lambda e: e.reciprocal(out, in_), [out], [in_])

    def rmax(self, out, in_):
        return self.P.add("dve", lambda e: e.reduce_max(out, in_, AX.X), [out], [in_])

    def rsum(self, out, in_):
        return self.P.add("dve", lambda e: e.reduce_sum(out, in_, AX.X), [out], [in_])

    def memset(self, ap, val, eng="dve"):
        return self.P.add(eng, lambda e: e.memset(ap, val), [ap], [])

    def dma(self, out, in_, key, eng="sp"):
        return self.P.add(eng, lambda e: e.dma_start(out=out, in_=in_), [out], [in_], dma_key=key)


C_ID, C_ONES, C_TRI, C_MNEG, C_STRICT, C_MLOW, C_EPS, C_N = 0, 128, 256, 320, 384, 448, 512, 520


def make_consts():
    c = np.zeros((128, C_N), np.float32)
    c[:, C_ID:C_ID + 128] = np.eye(128, dtype=np.float32)
    c[:, C_ONES:C_ONES + 128] = 1.0
    s = np.arange(64)[:, None]
    cc = np.arange(64)[None, :]
    c[:64, C_TRI:C_TRI + 64] = (s <= cc)
    c[:64, C_MNEG:C_MNEG + 64] = np.where(s <= cc, 0.0, NEG)
    c[:64, C_STRICT:C_STRICT + 64] = (s < cc)
    c[:64, C_MLOW:C_MLOW + 64] = np.where(cc <= s, 0.0, NEG)
    c[:, C_EPS] = EPS
    c[:, C_EPS + 1] = 1.0
    return c


INPUT_SPECS = [
    ("xT", [128, 8, SEQ], F32), ("xo", [128, 8, 2048], F32), ("cT", [128, 8], F32),
    ("wada", [128, 8, 8192], F32), ("bada", [128, 64], F32), ("norms", [128, 24], F32),
    ("win", [128, 8, D_IN], F32), ("dnconv", [128, 12, 4], F32), ("mlconv", [128, 4, 4], F32),
    ("hp", [128, 16], F32), ("dnnorm", [128, 1], F32), ("mlnorm", [128, 4], F32),
    ("wout", [128, 8, 1024], F32), ("wr", [128, 8, 32], F32), ("br", [128, 32], F32),
    ("wgu", [32, 1024, 2048], F32), ("bgu", [128, 32, 16], F32),
    ("wd", [32, 1024, 1024], F32), ("bd", [32, 1024], F32),
    ("consts", [128, C_N], F32), ("gidx", [128, 8], I32),
]


def build(stage=99, dbg=False):
    nc = bass.Bass("TRN2", target_bir_lowering=False)
    T = {}
    for name, shape, dtp in INPUT_SPECS:
        T[name] = nc.dram_tensor(name, shape, dtp, kind="ExternalInput").ap()
    outT = nc.dram_tensor("outT", [128, 8, 2048], F32, kind="ExternalOutput").ap()
    ybuf = nc.dram_tensor("ybuf", [8 * 128 * 2, 2048], BF16, kind="Internal").ap()
    T["x1buf"] = nc.dram_tensor("x1buf", [128, 8, 2048], F32, kind="Internal").ap()
    dbg_t = None
    if dbg:
        dbg_t = nc.dram_tensor("dbg", [128, 8192], F32, kind="ExternalOutput").ap()
    stack = ExitStack()
    with stack:
        k = K(nc, stack)
        P = k.P
        dbg_off = [0]

        def dump(name, ap):
            if not dbg:
                return
            p, n = ap.shape[0], ap.shape[1]
            DEBUG[name] = (dbg_off[0], p, n)
            if ap.dtype != F32:
                k.push()
                tmpd = k.alloc([n], F32)
                k.copy(tmpd[0:p], ap)
                k.dma(dbg_t[0:p, dbg_off[0]:dbg_off[0] + n], tmpd[0:p], "dbg")
                k.pop()
                dbg_off[0] += n
                return
            k.dma(dbg_t[0:p, dbg_off[0]:dbg_off[0] + n], ap, "dbg")
            dbg_off[0] += n

        consts = k.alloc([C_N], F32)
        k.dma(consts, T["consts"], "c0")
        ident = consts[:, C_ID:C_ID + 128]
        ones = consts[:, C_ONES:C_ONES + 128]
        epsc = consts[:, C_EPS:C_EPS + 1]
        ones_bf = k.alloc([128], BF16)
        k.copy(ones_bf, ones)
        ident_bf = k.alloc([128], BF16)
        k.copy(ident_bf, ident)
        norms = k.alloc([24], F32)
        k.dma(norms, T["norms"], "c0")
        mod = k.alloc([64], F32)
        modw = k.alloc([24], F32)

        k.push()
        cT = k.alloc([8], F32)
        k.dma(cT, T["cT"], "c0")
        bada = k.alloc([64], F32)
        k.dma(bada, T["bada"], "c0")
        cond = k.alloc([8], F32)
        k.act(cond, cT, AF.Silu)
        wbuf = [k.alloc([8, 1024], F32) for _ in range(2)]
        modps = k.ps(128)
        for blk in range(8):
            wb = wbuf[blk % 2]
            k.dma(wb, T["wada"][:, :, blk * 1024:(blk + 1) * 1024], "wa%d" % (blk % 2))
            for jj in range(8):
                j = blk * 8 + jj
                for kc in range(8):
                    k.mm(modps[:, j:j + 1], wb[:, kc, jj * 128:(jj + 1) * 128], cond[:, kc:kc + 1],
                         start=(kc == 0), stop=(kc == 7))
        k.tt(mod, modps[:, 0:64], bada, ALU.add)
        k.stt(modw[:, 0:8], mod[:, 8:16], 1.0, norms[:, 0:8], ALU.add, ALU.mult)
        k.stt(modw[:, 8:16], mod[:, 32:40], 1.0, norms[:, 8:16], ALU.add, ALU.mult)
        k.stt(modw[:, 16:24], mod[:, 56:64], 1.0, norms[:, 16:24], ALU.add, ALU.mult)
        k.pop()
        SH1, G1, SH2, G2, SHF = mod[:, 0:8], mod[:, 16:24], mod[:, 24:32], mod[:, 40:48], mod[:, 48:56]
        W1, W2, WF = modw[:, 0:8], modw[:, 8:16], modw[:, 16:24]
        dump("mod", mod)
        dump("modw", modw)

        def norm_block(xb, n, wv, shv, hT_out):
            k.push()
            sq = k.alloc([8, n], BF16)
            k.act(sq, xb, AF.Square)
            ss = k.ps(n)
            for kc in range(8):
                k.mm(ss, ones_bf, sq[:, kc, :], start=(kc == 0), stop=(kc == 7))
            rstd = k.alloc([n], F32)
            k.act(rstd, ss, AF.Sqrt, bias=epsc, scale=1.0 / D)
            k.recip(rstd, rstd)
            tmp = k.alloc([2, n], F32)
            for kc in range(8):
                k.tt(tmp[:, kc % 2, :], xb[:, kc, :], rstd, ALU.mult)
                k.act(hT_out[:, kc, :], tmp[:, kc % 2, :], AF.Identity, bias=shv[:, kc:kc + 1], scale=wv[:, kc:kc + 1])
            k.pop()

        if stage >= 1:
            mixer_phase(k, T, dict(ident=ident, ones=ones, ones_bf=ones_bf, ident_bf=ident_bf, consts=consts,
                                   epsc=epsc, W1=W1, SH1=SH1), norm_block, ybuf, dump, stage)

        if stage >= 4:
            post_phase(k, nc, T, dict(ident=ident, ones=ones, ones_bf=ones_bf, consts=consts, epsc=epsc,
                                      W2=W2, SH2=SH2, G1=G1, G2=G2, WF=WF, SHF=SHF), norm_block, ybuf, outT, dump)

        fin_ins = [outT]
        if dbg:
            fin_ins.append(dbg_t)
        P.add("sp", lambda e: e.nop(), [], fin_ins)
        cnt = P.emit(nc, stack)
    return nc, cnt


FM = ([("dq%d" % h, h * 128) for h in range(4)] + [("dk%d" % h, 512 + h * 128) for h in range(4)] +
      [("dv%d" % h, 1024 + h * 128) for h in range(4)] + [("mq%d" % i, 2056 + i * 128) for i in range(2)] +
      [("mk%d" % i, 2312 + i * 128) for i in range(2)] + [("dz%d" % h, 1536 + h * 128) for h in range(4)] +
      [("mo%d" % h, 3080 + h * 128) for h in range(4)])
FMI = {n: i for i, (n, _) in enumerate(FM)}
NCONV = 16
BLK = 256


def mixer_phase(k, T, C, norm_block, ybuf, dump, stage):
    ident, ones, ones_bf, consts, epsc = C["ident"], C["ones"], C["ones_bf"], C["consts"], C["epsc"]
    onec = consts[:, C_EPS + 1:C_EPS + 2]
    tri = consts[0:64, C_TRI:C_TRI + 64]
    mneg = consts[0:64, C_MNEG:C_MNEG + 64]
    strict = consts[0:64, C_STRICT:C_STRICT + 64]
    mlow = consts[0:64, C_MLOW:C_MLOW + 64]
    id64 = ident[0:64, 0:64]
    k.push()
    win_bf = k.alloc([8, D_IN], BF16)
    k.push()
    stg = [k.alloc([8, 450], F32) for _ in range(2)]
    for i in range(8):
        s = stg[i % 2]
        k.dma(s, T["win"][:, :, i * 450:(i + 1) * 450], "ws%d" % (i % 2))
        k.copy(win_bf[:, :, i * 450:(i + 1) * 450], s, eng=("act" if i % 2 else "dve"))
    k.pop()
    dnconv = k.alloc([12, 4], F32)
    k.dma(dnconv, T["dnconv"], "c1")
    mlconv = k.alloc([4, 4], F32)
    k.dma(mlconv, T["mlconv"], "c1")
    hp = k.alloc([16], F32)
    k.dma(hp, T["hp"], "c1")
    dnnorm = k.alloc([1], F32)
    k.dma(dnnorm, T["dnnorm"], "c1")
    mlnorm = k.alloc([4], F32)
    k.dma(mlnorm, T["mlnorm"], "c1")
    nega = k.alloc([4], F32)
    k.act(nega, hp[:, 0:4], AF.Exp)
    k.ts(nega, nega, -1.0, None, ALU.mult)

    xb = k.alloc([8, BLK], F32)
    hT = k.alloc([8, BLK], BF16)
    pb = k.alloc([len(FM), 3 + BLK], F32)
    cv = k.alloc([NCONV, BLK], F32)
    cvb = k.alloc([NCONV, BLK], BF16)
    gz = k.alloc([8, BLK], F32)
    ytile = k.alloc([8, BLK], BF16)
    k.memset(pb[:, :, 0:3], 0.0)

    def A(shape, dtp):
        return k.alloc(shape, dtp)
    GD = []
    for h in range(4):
        d = dict(S=A([128], F32), Sb=A([128], BF16), grep=A([128], F32), brep=A([64], F32), Dm=A([64], F32),
                 E=A([64], F32), Bs=A([64], F32), M=A([64], F32), MT=A([64], F32), U=A([64], F32),
                 QK=A([64], BF16), Pa=A([64], F32), PTa=A([64], F32), Pb=A([64], F32), PTb=A([64], F32),
                 kb=A([128], F32), kdec=A([128], BF16), vb=A([128], F32), w=A([128], F32),
                 kcT=A([64], BF16), eGb=A([64], F32), qdT=A([64], BF16), vnew=A([128], BF16),
                 osq=A([64], BF16), rn=A([64], F32), y=A([64], F32))
        k.memset(d["S"], 0.0)
        k.memset(d["Sb"], 0.0)
        GD.append(d)
    ML = []
    for h in range(4):
        d = dict(CN=A([256], F32), CNb=A([256], BF16), ms=A([1], F32), lrep=A([128], F32), nrep=A([128], F32),
                 bb=A([64], F32), nab=A([64], F32), amax=A([1], F32), dcs=A([64], F32), cmx=A([1], F32),
                 mto=A([1], F32), mrep=A([128], F32), tmpm=A([64], F32), D2=A([64], F32), pT=A([64], BF16),
                 inter=A([64], F32), qiT=A([64], BF16), flo=A([64], F32), dn=A([64], F32), hT=A([64], F32),
                 hsq=A([64], BF16), rn=A([64], F32), mx=A([1], F32), keep=A([1], F32), ew=A([1], F32),
                 kw=A([128], BF16))
        k.memset(d["CN"], 0.0)
        k.memset(d["CNb"], 0.0)
        k.memset(d["ms"], 0.0)
        k.memset(d["kw"], 0.0)
        ML.append(d)
    gtm = A([16], F32)
    gt = dict(beta=A([4], F32), x=A([4], F32), ax=A([4], F32), e=A([4], F32), r=A([4], F32), g=A([4], F32),
              G=A([4], F32), Gl=A([4], F32), ebg=A([4], F32), edl=A([4], F32),
              ip=A([4], F32), xf=A([4], F32), lf=A([4], F32), b=A([4], F32), na=A([4], F32))
    vaug = A([4, 256], BF16)
    k.memset(vaug[:, :, 128:256], 1.0)
    ktm = [A([128], F32) for _ in range(2)]
    kz = [A([BLK], BF16) for _ in range(4)]
    for h in range(4):
        k.memset(kz[h], 0.0)

    def softplus(out, x, P_=64):
        k.stt(gt["ax"][0:P_], x, -1.0, x, ALU.mult, ALU.max)
        k.act(gt["e"][0:P_], gt["ax"][0:P_], AF.Exp, scale=-1.0)
        k.act(gt["e"][0:P_], gt["e"][0:P_], AF.Ln, bias=onec[0:P_], scale=1.0)
        k.ts(gt["r"][0:P_], x, 0.0, None, ALU.max)
        k.tt(out, gt["r"][0:P_], gt["e"][0:P_], ALU.add)

    nblk = SEQ // BLK if stage >= 3 else 1
    if SIM_NBLK:
        nblk = SIM_NBLK
    for blk in range(nblk):
        t0 = blk * BLK
        if blk > 0:
            k.copy(pb[:, :, 0:3], pb[:, :, BLK:BLK + 3], eng="pool")
        k.dma(xb, T["xT"][:, :, t0:t0 + BLK], "xb")
        norm_block(xb, BLK, C["W1"], C["SH1"], hT)
        if blk == 0:
            dump("hT0", hT[:, 0, :])
        for ci, (nm, c0) in enumerate(FM):
            ps = k.ps(BLK)
            for kc in range(8):
                k.mm(ps, win_bf[:, kc, c0:c0 + 128], hT[:, kc, :], start=(kc == 0), stop=(kc == 7))
            k.copy(pb[:, ci, 3:3 + BLK], ps, eng=("act" if ci % 2 else "dve"))
        if blk == 0:
            dump("pq0", pb[:, FMI["dq0"], 3:3 + BLK])
            dump("mo3", pb[:, FMI["mo3"], 3:3 + BLK])
        if stage < 2:
            continue
        for ci in range(NCONV):
            wv = dnconv[:, ci, :] if ci < 12 else mlconv[:, ci - 12, :]
            k.ts(cv[:, ci, :], pb[:, ci, 0:BLK], wv[:, 0:1], None, ALU.mult, eng="pool")
            for j in range(1, 4):
                k.stt(cv[:, ci, :], pb[:, ci, j:j + BLK], wv[:, j:j + 1], cv[:, ci, :], ALU.mult, ALU.add)
            k.act(cv[:, ci, :], cv[:, ci, :], AF.Silu)
        if SUB == 1:
            dump("cvA", cv[:, 0, :])
            return
        for ci in range(8):
            sq = cvb[:, ci, :]
            k.act(sq, cv[:, ci, :], AF.Square)
            ss = k.ps(BLK)
            k.mm(ss, ones_bf, sq)
            rn = gz[:, 0, :]
            k.act(rn, ss, AF.Sqrt, bias=epsc, scale=1.0)
            k.recip(rn, rn)
            if ci < 4:
                k.stt(cv[:, ci, :], cv[:, ci, :], 128.0 ** -0.5, rn, ALU.mult, ALU.mult)
            else:
                k.tt(cv[:, ci, :], cv[:, ci, :], rn, ALU.mult)
        for ci in (12, 13):
            k.ts(cv[:, ci, :], cv[:, ci, :], 0.125, None, ALU.mult, eng="pool")
        k.copy(cvb, cv, eng="pool")
        for h in range(4):
            p0_ = (h % 2) * 64
            k.copy(kz[h][p0_:p0_ + 64, :], cv[p0_:p0_ + 64, 14 + h // 2, :], eng="pool")
        for h in range(4):
            k.act(gz[:, h, :], pb[:, FMI["dz%d" % h], 3:3 + BLK], AF.Silu)
            k.act(gz[:, 4 + h, :], pb[:, FMI["mo%d" % h], 3:3 + BLK], AF.Sigmoid)
        if blk == 0:
            dump("k0n", cv[:, 4, :])
            dump("mq0", cv[:, 12, :])

        if SUB == 2:
            return
        for j in range(EXP.get("nj", BLK // CH)):
            c0 = j * CH
            cs = slice(c0, c0 + CH)
            gps = k.ps(128)
            for kc in range(8):
                k.mm(gps[0:64, 0:8], hT[:, kc, cs], win_bf[:, kc, 2048:2056], start=(kc == 0), stop=(kc == 7))
            for kc in range(8):
                k.mm(gps[0:64, 8:16], hT[:, kc, cs], win_bf[:, kc, 3592:3600], start=(kc == 0), stop=(kc == 7))
            k.copy(gtm[0:64], gps[0:64, 0:16])
            vps = k.ps(512)
            for kc in range(8):
                k.mm(vps[0:64, :], hT[:, kc, cs], win_bf[:, kc, 2568:3080], start=(kc == 0), stop=(kc == 7))
            k.copy(vaug[0:64, :, 0:128], vps[0:64, :].rearrange("p (h e) -> p h e", h=4), eng="act")
            g = gt
            k.act(g["beta"][0:64], gtm[0:64, 0:4], AF.Sigmoid)
            k.tt(g["x"][0:64], gtm[0:64, 4:8], hp[0:64, 4:8], ALU.add)
            softplus(g["g"][0:64], g["x"][0:64])
            k.tt(g["g"][0:64], g["g"][0:64], nega[0:64], ALU.mult)
            Gps = k.ps(128)
            k.mm(Gps[0:64, 0:4], tri, g["g"][0:64])
            k.mm(Gps[0:64, 4:8], ones[0:64, 0:64], g["g"][0:64])
            k.copy(g["G"][0:64], Gps[0:64, 0:4])
            k.tt(g["edl"][0:64], Gps[0:64, 4:8], g["G"][0:64], ALU.subtract)
            k.act(g["edl"][0:64], g["edl"][0:64], AF.Exp)
            k.act(g["ebg"][0:64], g["G"][0:64], AF.Exp)
            k.tt(g["ebg"][0:64], g["ebg"][0:64], g["beta"][0:64], ALU.mult)
            k.tt(g["ip"][0:64], gtm[0:64, 8:12], hp[0:64, 8:12], ALU.add)
            k.tt(g["xf"][0:64], gtm[0:64, 12:16], hp[0:64, 12:16], ALU.add)
            k.ts(g["xf"][0:64], g["xf"][0:64], -1.0, None, ALU.mult)
            softplus(g["lf"][0:64], g["xf"][0:64])
            k.ts(g["lf"][0:64], g["lf"][0:64], -1.0, None, ALU.mult)
            bps = k.ps(128)
            k.mm(bps[0:64, 0:4], tri, g["lf"][0:64])
            k.copy(g["b"][0:64], bps[0:64, 0:4])
            k.tt(g["na"][0:64], g["ip"][0:64], g["b"][0:64], ALU.subtract)
            if blk == 0 and j == 0:
                dump("G0", g["G"][0:64])
                dump("b0", g["b"][0:64])
                dump("gtm", gtm[0:64])
                dump("hp", hp[0:64])
                dump("ip_a", g["ip"][0:64])
                dump("lf_a", g["lf"][0:64])

            if SUB == 3:
                return
            for pr in range(2):
                tkp = k.ps(128)
                k.tr(tkp[0:64, :], cv[:, 14 + pr, cs], ident)
                k.copy(ktm[pr][0:64], tkp[0:64, :], eng="act")
            def gdn_head(h):
                d = GD[h]
                bank = k.psum[:, h * 512:(h + 1) * 512]
                RA, RB, RC = bank[:, 0:128], bank[:, 128:256], bank[:, 256:512]
                kT, kTb = cv[:, 4 + h, cs], cvb[:, 4 + h, cs]
                qT, qTb = cv[:, h, cs], cvb[:, h, cs]
                vT = cv[:, 8 + h, cs]
                k.copy(d["grep"][0:64], g["g"][0:64, h:h + 1].to_broadcast([64, 128]), eng="pool")
                yield
                k.copy(d["brep"][0:64], g["beta"][0:64, h:h + 1].to_broadcast([64, 64]), eng="pool")
                yield
                Gb = RA
                k.mm(Gb[:, 0:64], d["grep"][0:64], tri)
                yield
                k.mm(Gb[0:64, 64:128], d["brep"][0:64], id64)
                yield
                k.act(d["eGb"], Gb[:, 0:64], AF.Exp)
                yield
                sc = RB
                k.mm(sc[0:64, 0:64], kTb, kTb)
                yield
                k.mm(sc[0:64, 64:128], kTb, qTb)
                yield
                k.stt(d["Dm"][0:64], Gb[0:64, 0:64], g["G"][0:64, h:h + 1], mneg, ALU.subtract, ALU.add)
                yield
                k.act(d["E"][0:64], d["Dm"][0:64], AF.Exp)
                yield
                k.tt(d["Bs"][0:64], Gb[0:64, 64:128], strict, ALU.mult)
                yield
                k.tt(d["M"][0:64], sc[0:64, 0:64], d["E"][0:64], ALU.mult)
                yield
                k.tt(d["M"][0:64], d["M"][0:64], d["Bs"][0:64], ALU.mult)
                yield
                k.tt(d["QK"][0:64], sc[0:64, 64:128], d["E"][0:64], ALU.mult)
                yield
                tp = RC
                k.tr(tp[0:64, 0:64], d["M"][0:64], id64)
                yield
                k.copy(d["MT"][0:64], tp[0:64, 0:64], eng=("dve" if EXP.get("mtdve") else "act"))
                yield
                k.tt(d["U"][0:64], id64, d["M"][0:64], ALU.subtract, eng="pool")
                yield
                Pc, PTc = d["M"], d["MT"]
                bufs = [(d["Pa"], d["PTa"]), (d["Pb"], d["PTb"])]
                for lev in range(1, EXP.get("nlev", 5) + 1):
                    Pn, PTn = bufs[lev % 2]
                    pp = RC
                    if EXP.get("v") == 1:
                        k.mm(pp[0:64, 0:64], Pc[0:64], Pc[0:64])
                        yield
                        k.copy(PTn[0:64], pp[0:64, 0:64])
                        yield
                        return
                    if EXP.get("v") == 2:
                        k.mm(pp[0:64, 0:64], id64, Pc[0:64])
                        yield
                        k.copy(PTn[0:64], pp[0:64, 0:64])
                        yield
                        return
                    if EXP.get("v") == 4:
                        k.mm(pp[0:64, 0:64], Pc[0:64], PTc[0:64])
                        yield
                        k.copy(PTn[0:64], pp[0:64, 0:64])
                        yield
                        return
                    if EXP.get("v") == 6:
                        k.mm(pp[0:64, 0:64], Pc[0:64], PTc[0:64])
                        yield
                        k.mm(pp[0:64, 64:128], PTc[0:64], Pc[0:64])
                        yield
                        k.copy(PTn[0:64], pp[0:64, 0:64])
                        yield
                        k.copy(Pn[0:64], pp[0:64, 64:128])
                        yield
                        return
                    if EXP.get("v") == 7:
                        k.mm(pp[0:64, 0:64], PTc[0:64], Pc[0:64])
                        yield
                        k.copy(PTn[0:64], pp[0:64, 0:64])
                        yield
                        return
                    if EXP.get("v") == 3:
                        k.mm(pp[0:64, 0:64], Pc[0:64], id64)
                        yield
                        k.copy(PTn[0:64], pp[0:64, 0:64])
                        yield
                        return
                    k.mm(pp[0:64, 0:64], Pc[0:64], PTc[0:64])
                    yield
                    if lev < 5:
                        k.mm(pp[0:64, 64:128], PTc[0:64], Pc[0:64])
                        yield
                    k.copy(PTn[0:64], pp[0:64, 0:64], eng="act")
                    yield
                    if lev < 5:
                        k.copy(Pn[0:64], pp[0:64, 64:128])
                        yield
                    if EXP.get("noup"):
                        Pc, PTc = Pn, PTn
                        continue
                    up = RA
                    k.mm(up[0:64, 0:64], PTn[0:64], d["U"][0:64])
                    yield
                    k.tt(d["U"][0:64], d["U"][0:64], up[0:64, 0:64], ALU.add)
                    yield
                    Pc, PTc = Pn, PTn
                tk = RC
                k.tr(tk[0:64, 0:128], kT, ident)
                yield
                k.tr(tk[0:64, 128:256], vT, ident)
                yield
                k.ts(d["kb"][0:64], tk[0:64, 0:128], g["ebg"][0:64, h:h + 1], None, ALU.mult)
                yield
                k.ts(d["kdec"][0:64], tk[0:64, 0:128], g["edl"][0:64, h:h + 1], None, ALU.mult)
                yield
                k.ts(d["vb"][0:64], tk[0:64, 128:256], g["beta"][0:64, h:h + 1], None, ALU.mult)
                yield
                wk = RC
                k.mm(wk[0:64, 0:128], d["U"][0:64], d["vb"][0:64])
                yield
                k.mm(wk[:, 128:192], d["kb"][0:64], d["U"][0:64])
                yield
                k.copy(d["w"][0:64], wk[0:64, 0:128], eng="act")
                yield
                k.copy(d["kcT"], wk[:, 128:192])
                yield
                k.tt(d["qdT"], qT, d["eGb"], ALU.mult, eng="pool")
                yield
                p1 = RA
                k.mm(p1[0:64, :], d["kcT"], d["Sb"])
                yield
                k.tt(d["vnew"][0:64], d["w"][0:64], p1[0:64, :], ALU.subtract)
                yield
                op_ = RB
                k.mm(op_[:, 0:64], d["Sb"], d["qdT"], start=True, stop=False)
                yield
                k.mm(op_[:, 0:64], d["vnew"][0:64], d["QK"][0:64], start=False, stop=True)
                yield
                dS = RC[:, 0:128]
                k.mm(dS, d["kdec"][0:64], d["vnew"][0:64])
                yield
                k.stt(d["S"], d["S"], d["eGb"][:, 63:64], dS, ALU.mult, ALU.add)
                yield
                k.copy(d["Sb"], d["S"], eng="act")
                yield
                k.act(d["osq"], op_[:, 0:64], AF.Square)
                yield
                ss = RA
                k.mm(ss[:, 0:64], ones_bf, d["osq"])
                yield
                k.act(d["rn"], ss[:, 0:64], AF.Sqrt, bias=epsc, scale=1.0 / 128)
                yield
                k.recip(d["rn"], d["rn"])
                yield
                k.stt(d["y"], op_[:, 0:64], dnnorm[:, 0:1], d["rn"], ALU.mult, ALU.mult)
                yield
                k.tt(ytile[:, h, cs], d["y"], gz[:, h, cs], ALU.mult, eng="pool")
                yield
                if blk == 0 and j == 0 and h == 0:
                    dump("M0", d["M"][0:64])
                    dump("U0", d["U"][0:64])
                    dump("y0", d["y"])

            def ml_head(h):
                d = ML[h]
                bank = k.psum[:, (4 + h) * 512:(5 + h) * 512]
                RA, RB, RC = bank[:, 0:128], bank[:, 128:256], bank[:, 256:512]
                pr, p0 = h // 2, (h % 2) * 64
                psl = slice(p0, p0 + 64)
                qT = cv[psl, 12 + pr, cs]
                qTb, kTb = cvb[psl, 12 + pr, cs], cvb[psl, 14 + pr, cs]
                k.copy(d["lrep"][0:64], g["lf"][0:64, h:h + 1].to_broadcast([64, 128]), eng="pool")
                yield
                k.copy(d["nrep"][0:64], g["na"][0:64, h:h + 1].to_broadcast([64, 128]), eng="pool")
                yield
                bp = RA
                k.mm(bp[:, 0:64], d["lrep"][0:64], tri)
                yield
                k.mm(bp[:, 64:128], d["nrep"][0:64], id64)
                yield
                k.copy(d["bb"], bp[:, 0:64], eng="act")
                yield
                k.copy(d["nab"], bp[:, 64:128])
                yield
                k.rmax(d["amax"], d["nab"])
                yield
                k.tt(d["dcs"][0:64], d["nab"][0:64], mlow, ALU.add, eng="pool")
                yield
                k.rmax(d["cmx"][0:64], d["dcs"][0:64])
                yield
                k.tt(d["mto"][0:64], d["cmx"][0:64], d["ms"][0:64], ALU.max)
                yield
                k.copy(d["mrep"][0:64], d["mto"][0:64, 0:1].to_broadcast([64, 128]), eng="pool")
                yield
                mp = RB
                k.mm(mp[:, 0:64], d["mrep"][0:64], id64)
                yield
                sc = RC
                k.mm(sc[0:64, 0:64], kz[h][:, cs], cvb[:, 12 + pr, cs])
                yield
                k.ts(d["tmpm"][0:64], mneg, g["na"][0:64, h:h + 1], None, ALU.add, eng="pool")
                yield
                k.stt(d["D2"][0:64], mp[0:64, 0:64], -1.0, d["tmpm"][0:64], ALU.mult, ALU.add)
                yield
                k.act(d["D2"][0:64], d["D2"][0:64], AF.Exp)
                yield
                k.tt(d["pT"][0:64], sc[0:64, 0:64], d["D2"][0:64], ALU.mult)
                yield
                k.act(d["inter"], mp[:, 0:64], AF.Exp, bias=d["ms"], scale=-1.0)
                yield
                k.tt(d["qiT"], cv[:, 12 + pr, cs], d["inter"], ALU.mult, eng="pool")
                yield
                k.tt(d["flo"], d["bb"], mp[:, 0:64], ALU.add)
                yield
                k.act(d["flo"], d["flo"], AF.Exp, scale=-1.0)
                yield
                nd = RC[:, 0:128]
                k.mm(nd[:, 0:64], d["CNb"][:, 0:128], d["qiT"], start=True, stop=False)
                yield
                k.mm(nd[:, 0:64], vaug[0:64, h, 0:128], d["pT"][0:64], start=False, stop=True)
                yield
                k.mm(nd[:, 64:128], d["CNb"][:, 128:256], d["qiT"], start=True, stop=False)
                yield
                k.mm(nd[:, 64:128], vaug[0:64, h, 128:256], d["pT"][0:64], start=False, stop=True)
                yield
                k.ts(d["dn"], nd[:, 64:128], -1.0, None, ALU.mult)
                yield
                k.tt(d["dn"], d["dn"], nd[:, 64:128], ALU.max)
                yield
                k.tt(d["dn"], d["dn"], d["flo"], ALU.max)
                yield
                k.recip(d["dn"], d["dn"])
                yield
                k.tt(d["hT"], nd[:, 0:64], d["dn"], ALU.mult)
                yield
                k.act(d["hsq"], d["hT"], AF.Square)
                yield
                ss = RA
                k.mm(ss[:, 0:64], ones_bf, d["hsq"])
                yield
                k.act(d["rn"], ss[:, 0:64], AF.Sqrt, bias=epsc, scale=1.0 / 128)
                yield
                k.recip(d["rn"], d["rn"])
                yield
                k.stt(d["hT"], d["hT"], mlnorm[:, h:h + 1], d["rn"], ALU.mult, ALU.mult)
                yield
                k.tt(ytile[:, 4 + h, cs], d["hT"], gz[:, 4 + h, cs], ALU.mult, eng="pool")
                yield
                k.tt(d["mx"], d["ms"], d["amax"], ALU.max)
                yield
                k.tt(d["keep"], d["ms"], d["mx"], ALU.subtract, eng="pool")
                yield
                k.act(d["keep"], d["keep"], AF.Exp)
                yield
                k.tt(d["ew"][0:64], g["na"][0:64, h:h + 1], d["mx"][0:64], ALU.subtract)
                yield
                k.act(d["ew"][0:64], d["ew"][0:64], AF.Exp)
                yield
                k.ts(d["kw"][0:64, p0:p0 + 64], ktm[pr][0:64, p0:p0 + 64], d["ew"][0:64, 0:1], None, ALU.mult)
                yield
                dc = RC
                k.mm(dc, d["kw"][0:64], vaug[0:64, h, :])
                yield
                k.stt(d["CN"], d["CN"], d["keep"][:, 0:1], dc, ALU.mult, ALU.add)
                yield
                k.copy(d["CNb"], d["CN"], eng="act")
                yield
                k.tt(d["ms"], d["bb"][:, 63:64], d["mx"], ALU.add, eng="pool")
                yield
                if blk == 0 and j == 0 and h == 0:
                    dump("mh0", d["hT"])
                    dump("nab", d["nab"])
                    dump("lrep", d["lrep"][0:64])
                    dump("nrep", d["nrep"][0:64])
                    dump("lf", g["lf"][0:64])
                    dump("na", g["na"][0:64])
                    dump("ip", g["ip"][0:64])
                    dump("bb", d["bb"])
                    dump("inter", d["inter"])
                    dump("flo", d["flo"])
                    dump("dn", d["dn"])
                    dump("pT", d["pT"][0:64])
                    dump("qiT", d["qiT"][0:64])
                    dump("rnm", d["rn"])

            gens = [gdn_head(h) for h in range(4)] + [ml_head(h) for h in range(4)]
            while gens:
                for g_ in list(gens):
                    try:
                        next(g_)
                    except StopIteration:
                        gens.remove(g_)
        if SUB == 5:
            return
        for hh in range(0 if EXP.get("noyb") else 8):
            half, off = t0 // 2048, t0 % 2048
            r0 = (half * 8 + hh) * 128
            k.dma(ybuf[r0:r0 + 128, off:off + BLK], ytile[:, hh, :], "yb")
        if blk == 0:
            k.push()
            yd = k.alloc([BLK], F32)
            for hh in (0, 4):
                k.copy(yd, ytile[:, hh, :])
                dump("yt%d" % hh, yd)
            k.pop()
    k.pop()


def _kc(a):
    a = np.asarray(a, np.float32)
    return np.ascontiguousarray(a.reshape((8, 128) + a.shape[1:]).swapaxes(0, 1))


def prep_inputs(inp):
    f = np.float32
    x = np.asarray(inp["x"], f)
    consts = make_consts()
    w_all = np.concatenate([np.asarray(inp["w_ada"][0], f), np.asarray(inp["w_ada_final"], f)], axis=1)
    b_all = np.concatenate([np.asarray(inp["b_ada"][0], f), np.asarray(inp["b_ada_final"], f)])
    shared = {
        "wada": _kc(w_all),
        "bada": np.ascontiguousarray(b_all.reshape(64, 128).T),
        "norms": np.ascontiguousarray(np.concatenate(
            [np.asarray(inp[n], f).reshape(8, 128).T for n in ("norm_mix", "norm_ffn", "norm_final")], axis=1)),
        "win": _kc(inp["w_in"][0]),
        "dnconv": np.ascontiguousarray(np.asarray(inp["dn_conv"][0], f).reshape(4, 12, 128).transpose(2, 1, 0)),
        "mlconv": np.ascontiguousarray(np.asarray(inp["ml_conv"][0], f).reshape(4, 4, 128).transpose(2, 1, 0)),
        "hp": np.ascontiguousarray(np.broadcast_to(np.concatenate(
            [np.asarray(inp[n][0], f) for n in ("dn_a_log", "dn_dt_bias", "ml_i_bias", "ml_f_bias")])[None, :], (128, 16))),
        "dnnorm": np.ascontiguousarray(np.asarray(inp["dn_norm"][0], f).reshape(128, 1)),
        "mlnorm": np.ascontiguousarray(np.asarray(inp["ml_norm"][0], f).reshape(4, 128).T),
        "wout": _kc(inp["w_out"][0]),
        "wr": _kc(inp["w_router"][0]),
        "br": np.ascontiguousarray(np.broadcast_to(np.asarray(inp["b_router"][0], f)[None, :], (128, 32))),
        "wgu": np.ascontiguousarray(np.asarray(inp["w_gate_up"][0], f)),
        "bgu": np.ascontiguousarray(np.asarray(inp["b_gate_up"][0], f).reshape(32, 16, 128).transpose(2, 0, 1)),
        "wd": np.ascontiguousarray(np.asarray(inp["w_down"][0], f)),
        "bd": np.ascontiguousarray(np.asarray(inp["b_down"][0], f)),
        "consts": consts,
    }
    maps = []
    for c in range(8):
        b, hf = c // 2, c % 2
        xT = _kc(x[b].T)
        m = dict(shared)
        m["xT"] = xT
        m["xo"] = np.ascontiguousarray(xT[:, :, hf * 2048:(hf + 1) * 2048])
        m["cT"] = np.ascontiguousarray(np.asarray(inp["c"], f)[b].reshape(8, 128).T)
        p = np.arange(128, dtype=np.int32)[:, None]
        h = np.arange(8, dtype=np.int32)[None, :]
        m["gidx"] = np.ascontiguousarray(((hf * 8 + h) * 128 + p).astype(np.int32))
        maps.append(m)
    return maps


def post_phase(k, nc, T, C, norm_block, ybuf, outT, dump):
    ident, epsc = C["ident"], C["epsc"]
    P = k.P
    P.whole.add("ybuf")
    NT = 2048
    TB = 512
    k.push()
    gidx = k.alloc([8], I32)
    k.dma(gidx, T["gidx"], "c2")
    h2T = k.alloc([8, NT], BF16)
    for hh in range(8):
        P.add("pool", (lambda hh: lambda e: e.indirect_dma_start(
            out=h2T[:, hh, :], out_offset=None, in_=ybuf[:, :],
            in_offset=bass.IndirectOffsetOnAxis(ap=gidx[:, hh:hh + 1], axis=0)))(hh),
            [h2T[:, hh, :]], [ybuf, gidx[:, hh:hh + 1]], dma_key="yg")
    if PSUB == 1:
        k.push()
        yd_ = k.alloc([512], F32)
        k.copy(yd_, h2T[:, 0, 0:512])
        dump("yg0", yd_)
        k.pop()
        return
    GT = k.alloc([NT], F32)
    wr = k.alloc([8, 32], F32)
    k.dma(wr, T["wr"], "c2")
    br = k.alloc([32], F32)
    k.dma(br, T["br"], "c2")
    bd = k.alloc([1024], F32)
    k.dma(bd[0:32], T["bd"], "c2")
    bgu = k.alloc([32, 16], F32)
    k.dma(bgu, T["bgu"], "c2")
    stg = [k.alloc([2048], F32) for _ in range(2)]
    sti = [0]

    def load_cast(dst_bf, src, n):
        s_ = stg[sti[0] % 2]
        key = "st%d" % (sti[0] % 2)
        sti[0] += 1
        k.dma(s_[:, 0:n], src, key)
        k.copy(dst_bf, s_[:, 0:n], eng="act")

    k.push()
    wout_bf = k.alloc([8, 1024], BF16)
    for yc in range(8):
        load_cast(wout_bf[:, yc, :], T["wout"][:, yc, :], 1024)
    xob = k.alloc([8, TB], F32)
    x1b = k.alloc([8, TB], F32)
    h2f = k.alloc([8, TB], F32)
    lg = k.alloc([32], F32)
    mx8 = k.alloc([8], F32)
    nm1 = k.alloc([1], F32)
    msk = k.alloc([32], F32)
    ex = k.alloc([32], F32)
    den = k.alloc([1], F32)
    for tb in range(NT // TB):
        ts_ = slice(tb * TB, (tb + 1) * TB)
        k.dma(xob, T["xo"][:, :, ts_], "xo")
        for dc in range(8):
            ps = k.ps(TB)
            for yc in range(8):
                k.mm(ps, wout_bf[:, yc, dc * 128:(dc + 1) * 128], h2T[:, yc, ts_], start=(yc == 0), stop=(yc == 7))
            k.stt(x1b[:, dc, :], ps, C["G1"][:, dc:dc + 1], xob[:, dc, :], ALU.mult, ALU.add)
        k.dma(T["x1buf"][:, :, ts_], x1b, "x1w")
        if tb == 0:
            dump("x1", x1b[:, 0, :])
        norm_block(x1b, TB, C["W2"], C["SH2"], h2f)
        k.copy(h2T[:, :, ts_], h2f, eng="pool")
        for tt_ in range(TB // 128):
            tsl = slice(tt_ * 128, (tt_ + 1) * 128)
            lp = k.ps(128)
            for kc in range(8):
                k.mm(lp[:, 0:32], h2f[:, kc, tsl], wr[:, kc, :], start=(kc == 0), stop=(kc == 7))
            k.tt(lg, lp[:, 0:32], br, ALU.add)
            P.add("dve", lambda e: e.max(mx8, lg), [mx8], [lg])
            k.ts(msk, lg, mx8[:, 3:4], None, ALU.is_ge)
            k.ts(nm1, mx8[:, 0:1], -1.0, None, ALU.mult)
            k.act(ex, lg, AF.Exp, bias=nm1, scale=1.0)
            k.tt(ex, ex, msk, ALU.mult)
            k.rsum(den, ex)
            k.recip(den, den)
            k.ts(ex, ex, den[:, 0:1], None, ALU.mult)
            tp = k.ps(128)
            k.tr(tp[0:32, 0:128], ex, ident)
            g0 = tb * TB + tt_ * 128
            k.copy(GT[0:32, g0:g0 + 128], tp[0:32, 0:128], eng="act")
            if tb == 0 and tt_ == 0:
                dump("lg", lg)
                dump("gate", ex)
    k.pop()
    if PSUB == 2:
        return

    acc = k.alloc([8, NT], F32)
    k.push()
    wgu_bf = k.alloc([8, 2048], BF16)
    wd_bf = k.alloc([8, 1024], BF16)
    sel = k.alloc([128], F32)
    gbe = k.alloc([TB], F32)
    actT = k.alloc([8, TB], BF16)
    tg = [k.alloc([TB], F32) for _ in range(2)]
    tsg = [k.alloc([TB], F32) for _ in range(2)]
    tu = [k.alloc([TB], F32) for _ in range(2)]
    for tb in range(NT // TB):
        ts_ = slice(tb * TB, (tb + 1) * TB)
        for dc in range(8):
            ps = k.ps(TB)
            k.mm(ps, bd[0:32, dc * 128:(dc + 1) * 128], GT[0:32, ts_])
            k.copy(acc[:, dc, ts_], ps, eng=("act" if dc % 2 else "dve"))
    if EXP.get("m") == 0:
        return
    for kc in range(8):
        load_cast(wgu_bf[:, kc, :], T["wgu"][0, kc * 128:(kc + 1) * 128, :], 2048)
    if EXP.get("m") == 1:
        return
    NE = SIM_NE or 32
    it = 0
    for e in range(NE):
        for fc in range(8):
            load_cast(wd_bf[:, fc, :], T["wd"][e, fc * 128:(fc + 1) * 128, :], 1024)
        k.copy(sel[0:32], ident[0:32, e:e + 1].to_broadcast([32, 128]))
        for tb in range(NT // TB):
            ts_ = slice(tb * TB, (tb + 1) * TB)
            gp = k.ps(TB)
            k.mm(gp, sel[0:32], GT[0:32, ts_])
            k.copy(gbe, gp, eng="act")
            if EXP.get("m") == 2:
                return
            for fc in range(8):
                b = it % 2
                it += 1
                gps = k.ps(TB)
                for kc in range(8):
                    k.mm(gps, wgu_bf[:, kc, fc * 128:(fc + 1) * 128], h2T[:, kc, ts_], start=(kc == 0), stop=(kc == 7))
                ups = k.ps(TB)
                for kc in range(8):
                    k.mm(ups, wgu_bf[:, kc, 1024 + fc * 128:1024 + (fc + 1) * 128], h2T[:, kc, ts_],
                         start=(kc == 0), stop=(kc == 7))
                k.ts(tg[b], gps, bgu[:, e, fc:fc + 1], 7.0, ALU.add, ALU.min)
                k.act(tsg[b], tg[b], AF.Sigmoid, scale=1.702)
                k.ts(tu[b], ups, bgu[:, e, 8 + fc:9 + fc], 7.0, ALU.add, ALU.min)
                if EXP.get("m") == 3:
                    return
                k.ts(tu[b], tu[b], -7.0, 1.0, ALU.max, ALU.add)
                k.tt(tg[b], tg[b], tsg[b], ALU.mult)
                k.tt(tu[b], tu[b], gbe, ALU.mult, eng="pool")
                k.tt(actT[:, fc, :], tg[b], tu[b], ALU.mult, eng="pool")
                if EXP.get("m") == 4:
                    return
            if EXP.get("m") == 5:
                return
            if e + 1 < NE and tb == NT // TB - 1:
                for kc in range(8):
                    load_cast(wgu_bf[:, kc, :], T["wgu"][e + 1, kc * 128:(kc + 1) * 128, :], 2048)
            for dc in range(8):
                ps = k.ps(TB)
                for fc in range(8):
                    k.mm(ps, wd_bf[:, fc, dc * 128:(dc + 1) * 128], actT[:, fc, :], start=(fc == 0), stop=(fc == 7))
                k.tt(acc[:, dc, ts_], acc[:, dc, ts_], ps, ALU.add)
            if EXP.get("m") == 6:
                return
            if EXP.get("m") == 7 and tb == 1:
                return
            if EXP.get("m") == 8 and tb == 3:
                dump("acc", acc[:, 0, 0:512])
                return
    dump("acc", acc[:, 0, 0:512])
    k.pop()
    if PSUB == 3:
        return

    k.push()
    x1b = k.alloc([8, TB], F32)
    of = k.alloc([8, TB], F32)
    for tb in range(NT // TB):
        ts_ = slice(tb * TB, (tb + 1) * TB)
        k.dma(x1b, T["x1buf"][:, :, ts_], "x1r")
        for dc in range(8):
            k.stt(x1b[:, dc, :], acc[:, dc, ts_], C["G2"][:, dc:dc + 1], x1b[:, dc, :], ALU.mult, ALU.add)
        norm_block(x1b, TB, C["WF"], C["SHF"], of)
        k.dma(outT[:, :, ts_], of, "out")
    k.pop()
    k.pop()


_CACHE = {}


def kernel(**inputs):
    if "nc" not in _CACHE:
        _CACHE["nc"] = build(stage=99, dbg=False)[0]
    nc = _CACHE["nc"]
    maps = prep_inputs(inputs)
    res = run_bass_kernel_spmd(nc, maps, core_ids=list(range(8)))
    out = np.zeros((NB, SEQ, D), np.float32)
    for c in range(8):
        b, hf = c // 2, c % 2
        o = np.asarray(res.results[c]["outT"])
        out[b, hf * 2048:(hf + 1) * 2048, :] = o.transpose(2, 1, 0).reshape(2048, D)
    return out
```

```python
import numpy as np
import concourse.bass as bass
import concourse.mybir as mybir
from concourse.bass_utils import run_bass_kernel_spmd
from contextlib import ExitStack

dt = mybir.dt
F32, BF16, I32, U8 = dt.float32, dt.bfloat16, dt.int32, dt.uint8
ALU = mybir.AluOpType
AF = mybir.ActivationFunctionType
AX = mybir.AxisListType

D = 1024
SEQ = 4096
NB = 4
KC = 8
CH = 64
EPS = 1e-6
D_IN = 3600
NEG = -60000.0

DEBUG = {}
DEBUG_ON = False
SIM_NBLK = 0
SUB = 0
PSUB = 0
EXP = {}
SIM_NE = 0


def _dsize(d):
    if d == F32 or d == I32:
        return 4
    if d == BF16:
        return 2
    if d == U8:
        return 1
    raise ValueError(str(d))


def _rect(ap):
    a = ap.ap
    ds = _dsize(ap.dtype)
    name = ap.tensor.name
    if str(ap.space) == "DRAM":
        ext = sum((c - 1) * abs(s) for s, c in a)
        return (name, 0, 1, ap.offset * ds, (ap.offset + ext + 1) * ds)
    pstep, pcnt = a[0]
    if pstep == 0:
        pstep = 1 << 40
    p0 = ap.offset // pstep
    fo = ap.offset % pstep
    ext = sum((c - 1) * abs(s) for s, c in a[1:])
    b0, b1 = fo * ds, (fo + ext + 1) * ds
    p1 = p0 + pcnt
    if str(ap.space) == "PSUM":
        b0 = b0 // 2048 * 2048
        b1 = (b1 + 2047) // 2048 * 2048
        p0 = 0
        p1 = 128
    return (name, p0, p1, b0, b1)


class Op:
    __slots__ = ("eng", "fn", "deps", "dma_key", "dma_val", "count", "signal", "waits", "clock", "dma_snap", "idx")

    def __init__(self, eng, fn):
        self.eng = eng
        self.fn = fn
        self.deps = {}
        self.dma_key = None
        self.dma_val = 0
        self.count = 0
        self.signal = False
        self.waits = []
        self.clock = None
        self.dma_snap = None


ENGS = ("pe", "act", "dve", "pool", "sp")


class Prog:
    def __init__(self):
        self.ops = []
        self.eng_ops = {e: [] for e in ENGS}
        self.hist = {}
        self.dma_count = {}
        self.dma_waiters = {}
        self.whole = set()

    def _dep(self, op, prod):
        if prod is op:
            return
        if prod.eng == "pe" and op.eng == "pe" and prod.dma_key is None and op.dma_key is None:
            return
        op.deps[id(prod)] = prod

    def _access(self, op, ap, write):
        name, p0, p1, b0, b1 = _rect(ap)
        if name in self.whole:
            p0, p1, b0, b1 = 0, 1 << 30, 0, 1 << 60
        lst = self.hist.setdefault(name, [])
        keep = []
        ch = ("d", op.dma_key) if op.dma_key is not None else ("e", op.eng)
        for ent in lst:
            q0, q1, c0, c1, prod, w, pch = ent
            ov = (q0 < p1 and p0 < q1 and c0 < b1 and b0 < c1)
            if ov and (write or w):
                self._dep(op, prod)
            elif ov and name == "psum" and prod.eng != op.eng:
                self._dep(op, prod)
            if write and q0 >= p0 and q1 <= p1 and c0 >= b0 and c1 <= b1:
                continue
            if (not write) and (not w) and pch == ch and q0 == p0 and q1 == p1 and c0 == b0 and c1 == b1:
                continue
            keep.append(ent)
        keep.append((p0, p1, b0, b1, op, write, ch))
        self.hist[name] = keep

    def add(self, eng, fn, outs, ins, dma_key=None):
        op = Op(eng, fn)
        op.idx = len(self.ops)
        if dma_key is not None:
            op.dma_key = dma_key
            for w in self.dma_waiters.get(dma_key, []):
                self._dep(op, w)
            self.dma_waiters[dma_key] = []
        for ap in ins:
            self._access(op, ap, False)
        for ap in outs:
            self._access(op, ap, True)
        snap = {}
        for prod in op.deps.values():
            if prod.dma_key is not None:
                k = prod.dma_key
                snap[k] = 16 * self.dma_count[k]
                self.dma_waiters.setdefault(k, []).append(op)
        op.dma_snap = snap
        if dma_key is not None:
            self.dma_count[dma_key] = self.dma_count.get(dma_key, 0) + 1
            op.dma_val = 16 * self.dma_count[dma_key]
        self.ops.append(op)
        self.eng_ops[eng].append(op)
        return op

    def finalize(self):
        for op in self.ops:
            for prod in op.deps.values():
                if prod.dma_key is None:
                    prod.signal = True
        cnt = {e: 0 for e in ENGS}
        for op in self.ops:
            if op.dma_key is None and op.signal:
                cnt[op.eng] += 1
                op.count = cnt[op.eng]
        last_clock = {e: {} for e in ENGS}
        for op in self.ops:
            base = dict(last_clock[op.eng])
            needs = []
            for prod in op.deps.values():
                if prod.dma_key is not None:
                    needs.append((("d", prod.dma_key), op.dma_snap[prod.dma_key], prod))
                else:
                    needs.append((("e", prod.eng), prod.count, prod))
            needs.sort(key=lambda t: -t[2].idx)
            waits = []
            for ch, val, prod in needs:
                if base.get(ch, 0) >= val:
                    continue
                waits.append((ch, val))
                for k, v in prod.clock.items():
                    if base.get(k, 0) < v:
                        base[k] = v
                base[ch] = max(base.get(ch, 0), val)
            wd = {}
            for ch, val in waits:
                wd[ch] = max(wd.get(ch, 0), val)
            op.waits = list(wd.items())
            last_clock[op.eng] = base
            oc = dict(base)
            if op.dma_key is not None:
                oc[("d", op.dma_key)] = max(oc.get(("d", op.dma_key), 0), op.dma_val)
            elif op.signal:
                oc[("e", op.eng)] = max(oc.get(("e", op.eng), 0), op.count)
            op.clock = oc
        return cnt

    def emit(self, nc, stack):
        cnt = self.finalize()
        sems = {}
        for e in ENGS:
            sems[("e", e)] = stack.enter_context(nc.semaphore("s_" + e))
        for k in self.dma_count:
            sems[("d", k)] = stack.enter_context(nc.semaphore("d_" + str(k)))
        self.sems = sems
        block = stack.enter_context(nc.Block())
        eng_ops = self.eng_ops

        def run(engine, ops):
            for op in ops:
                for ch, val in op.waits:
                    engine.wait_ge(sems[ch], val)
                ins = op.fn(engine)
                if op.dma_key is not None:
                    ins.then_inc(sems[("d", op.dma_key)], 16)
                elif op.signal:
                    ins.then_inc(sems[("e", op.eng)], 1)

        @block.tensor
        def _(e):
            run(e, eng_ops["pe"])

        @block.scalar
        def _(e):
            run(e, eng_ops["act"])

        @block.vector
        def _(e):
            run(e, eng_ops["dve"])

        @block.gpsimd
        def _(e):
            run(e, eng_ops["pool"])

        @block.sync
        def _(e):
            run(e, eng_ops["sp"])
        return cnt


class K:
    def __init__(self, nc, stack):
        self.nc = nc
        self.P = Prog()
        self.arena_bytes = 206 * 1024
        self.arena = stack.enter_context(nc.sbuf_tensor("arena", [128, self.arena_bytes], U8))
        self.psum = stack.enter_context(nc.psum_tensor("psum", [128, 4096], F32))
        self.top = 0
        self.marks = []
        self.ps_small = 0
        self.ps_wide = 0
        self.dve_toggle = 0

    def alloc(self, shape, dtype):
        n = 1
        for s in shape:
            n *= s
        nbytes = n * _dsize(dtype)
        off = (self.top + 63) // 64 * 64
        assert off + nbytes <= self.arena_bytes, ("SBUF arena overflow", off, nbytes)
        self.top = off + nbytes
        v = self.arena[:, off:off + nbytes].bitcast(dtype)
        if len(shape) == 2:
            v = v.rearrange("p (a b) -> p a b", a=shape[0])
        elif len(shape) == 3:
            v = v.rearrange("p (a b c) -> p a b c", a=shape[0], b=shape[1])
        return v

    def push(self):
        self.marks.append(self.top)

    def pop(self):
        self.top = self.marks.pop()

    def ps(self, cols=128):
        b = self.ps_wide % 8
        self.ps_wide += 1
        return self.psum[:, b * 512:b * 512 + cols]

    def mm(self, out, lhsT, rhs, start=True, stop=True):
        return self.P.add("pe", lambda e: e.matmul(out, lhsT, rhs, start=start, stop=stop), [out], [lhsT, rhs])

    def tr(self, out, in_, ident):
        if EXP.get("notr", 1):
            return self.P.add("pe", lambda e: e.matmul(out, in_, ident, start=True, stop=True), [out], [in_, ident])
        return self.P.add("pe", lambda e: e.transpose(out, in_, ident), [out], [in_, ident])

    def act(self, out, in_, func, bias=None, scale=None, eng="act"):
        ins = [in_]
        kw = {}
        if bias is not None:
            kw["bias"] = bias
            if not isinstance(bias, (int, float)):
                ins.append(bias)
        if scale is not None:
            kw["scale"] = scale
            if not isinstance(scale, (int, float)):
                ins.append(scale)
        return self.P.add(eng, lambda e: e.activation(out, in_, func, **kw), [out], ins)

    def tt(self, out, in0, in1, op, eng="dve"):
        return self.P.add(eng, lambda e: e.tensor_tensor(out, in0, in1, op), [out], [in0, in1])

    def ts(self, out, in0, s1, s2, op0, op1=None, eng="dve"):
        ins = [in0]
        for s in (s1, s2):
            if s is not None and not isinstance(s, (int, float)):
                ins.append(s)
        if op1 is None:
            return self.P.add(eng, lambda e: e.tensor_scalar(out, in0, s1, None, op0), [out], ins)
        return self.P.add(eng, lambda e: e.tensor_scalar(out, in0, s1, s2, op0, op1), [out], ins)

    def stt(self, out, in0, scalar, in1, op0, op1, eng="dve"):
        ins = [in0, in1]
        if not isinstance(scalar, (int, float)):
            ins.append(scalar)
        return self.P.add(eng, lambda e: e.scalar_tensor_tensor(out, in0, scalar, in1, op0, op1), [out], ins)

    def copy(self, out, in_, eng="dve"):
        if eng == "act":
            return self.P.add("act", lambda e: e.copy(out, in_), [out], [in_])
        return self.P.add(eng, lambda e: e.tensor_copy(out, in_), [out], [in_])

    def recip(self, out, in_):
        return self.P.add("dve", lambda e: e.reciprocal(out, in_), [out], [in_])

    def rmax(self, out, in_):
        return self.P.add("dve", lambda e: e.reduce_max(out, in_, AX.X), [out], [in_])

    def rsum(self, out, in_):
        return self.P.add("dve", lambda e: e.reduce_sum(out, in_, AX.X), [out], [in_])

    def memset(self, ap, val, eng="dve"):
        return self.P.add(eng, lambda e: e.memset(ap, val), [ap], [])

    def dma(self, out, in_, key, eng="sp"):
        return self.P.add(eng, lambda e: e.dma_start(out=out, in_=in_), [out], [in_], dma_key=key)


C_ID, C_ONES, C_TRI, C_MNEG, C_STRICT, C_MLOW, C_EPS, C_N = 0, 128, 256, 320, 384, 448, 512, 520


def make_consts():
    c = np.zeros((128, C_N), np.float32)
    c[:, C_ID:C_ID + 128] = np.eye(128, dtype=np.float32)
    c[:, C_ONES:C_ONES + 128] = 1.0
    s = np.arange(64)[:, None]
    cc = np.arange(64)[None, :]
    c[:64, C_TRI:C_TRI + 64] = (s <= cc)
    c[:64, C_MNEG:C_MNEG + 64] = np.where(s <= cc, 0.0, NEG)
    c[:64, C_STRICT:C_STRICT + 64] = (s < cc)
    c[:64, C_MLOW:C_MLOW + 64] = np.where(cc <= s, 0.0, NEG)
    c[:, C_EPS] = EPS
    c[:, C_EPS + 1] = 1.0
    return c


INPUT_SPECS = [
    ("xT", [128, 8, SEQ], F32), ("xo", [128, 8, 2048], F32), ("cT", [128, 8], F32),
    ("wada", [128, 8, 8192], F32), ("bada", [128, 64], F32), ("norms", [128, 24], F32),
    ("win", [128, 8, D_IN], F32), ("dnconv", [128, 12, 4], F32), ("mlconv", [128, 4, 4], F32),
    ("hp", [128, 16], F32), ("dnnorm", [128, 1], F32), ("mlnorm", [128, 4], F32),
    ("wout", [128, 8, 1024], F32), ("wr", [128, 8, 32], F32), ("br", [128, 32], F32),
    ("wgu", [32, 1024, 2048], F32), ("bgu", [128, 32, 16], F32),
    ("wd", [32, 1024, 1024], F32), ("bd", [32, 1024], F32),
    ("consts", [128, C_N], F32), ("gidx", [128, 8], I32),
]


def build(stage=99, dbg=False):
    nc = bass.Bass("TRN2", target_bir_lowering=False)
    T = {}
    for name, shape, dtp in INPUT_SPECS:
        T[name] = nc.dram_tensor(name, shape, dtp, kind="ExternalInput").ap()
    outT = nc.dram_tensor("outT", [128, 8, 2048], F32, kind="ExternalOutput").ap()
    ybuf = nc.dram_tensor("ybuf", [8 * 128 * 2, 2048], BF16, kind="Internal").ap()
    T["x1buf"] = nc.dram_tensor("x1buf", [128, 8, 2048], F32, kind="Internal").ap()
    dbg_t = None
    if dbg:
        dbg_t = nc.dram_tensor("dbg", [128, 8192], F32, kind="ExternalOutput").ap()
    stack = ExitStack()
    with stack:
        k = K(nc, stack)
        P = k.P
        dbg_off = [0]

        def dump(name, ap):
            if not dbg:
                return
            p, n = ap.shape[0], ap.shape[1]
            DEBUG[name] = (dbg_off[0], p, n)
            if ap.dtype != F32:
                k.push()
                tmpd = k.alloc([n], F32)
                k.copy(tmpd[0:p], ap)
                k.dma(dbg_t[0:p, dbg_off[0]:dbg_off[0] + n], tmpd[0:p], "dbg")
                k.pop()
                dbg_off[0] += n
                return
            k.dma(dbg_t[0:p, dbg_off[0]:dbg_off[0] + n], ap, "dbg")
            dbg_off[0] += n

        consts = k.alloc([C_N], F32)
        k.dma(consts, T["consts"], "c0")
        ident = consts[:, C_ID:C_ID + 128]
        ones = consts[:, C_ONES:C_ONES + 128]
        epsc = consts[:, C_EPS:C_EPS + 1]
        ones_bf = k.alloc([128], BF16)
        k.copy(ones_bf, ones)
        ident_bf = k.alloc([128], BF16)
        k.copy(ident_bf, ident)
        norms = k.alloc([24], F32)
        k.dma(norms, T["norms"], "c0")
        mod = k.alloc([64], F32)
        modw = k.alloc([24], F32)

        k.push()
        cT = k.alloc([8], F32)
        k.dma(cT, T["cT"], "c0")
        bada = k.alloc([64], F32)
        k.dma(bada, T["bada"], "c0")
        cond = k.alloc([8], F32)
        k.act(cond, cT, AF.Silu)
        wbuf = [k.alloc([8, 1024], F32) for _ in range(2)]
        modps = k.ps(128)
        for blk in range(8):
            wb = wbuf[blk % 2]
            k.dma(wb, T["wada"][:, :, blk * 1024:(blk + 1) * 1024], "wa%d" % (blk % 2))
            for jj in range(8):
                j = blk * 8 + jj
                for kc in range(8):
                    k.mm(modps[:, j:j + 1], wb[:, kc, jj * 128:(jj + 1) * 128], cond[:, kc:kc + 1],
                         start=(kc == 0), stop=(kc == 7))
        k.tt(mod, modps[:, 0:64], bada, ALU.add)
        k.stt(modw[:, 0:8], mod[:, 8:16], 1.0, norms[:, 0:8], ALU.add, ALU.mult)
        k.stt(modw[:, 8:16], mod[:, 32:40], 1.0, norms[:, 8:16], ALU.add, ALU.mult)
        k.stt(modw[:, 16:24], mod[:, 56:64], 1.0, norms[:, 16:24], ALU.add, ALU.mult)
        k.pop()
        SH1, G1, SH2, G2, SHF = mod[:, 0:8], mod[:, 16:24], mod[:, 24:32], mod[:, 40:48], mod[:, 48:56]
        W1, W2, WF = modw[:, 0:8], modw[:, 8:16], modw[:, 16:24]
        dump("mod", mod)
        dump("modw", modw)

        def norm_block(xb, n, wv, shv, hT_out):
            k.push()
            sq = k.alloc([8, n], BF16)
            k.act(sq, xb, AF.Square)
            ss = k.ps(n)
            for kc in range(8):
                k.mm(ss, ones_bf, sq[:, kc, :], start=(kc == 0), stop=(kc == 7))
            rstd = k.alloc([n], F32)
            k.act(rstd, ss, AF.Sqrt, bias=epsc, scale=1.0 / D)
            k.recip(rstd, rstd)
            tmp = k.alloc([2, n], F32)
            for kc in range(8):
                k.tt(tmp[:, kc % 2, :], xb[:, kc, :], rstd, ALU.mult)
                k.act(hT_out[:, kc, :], tmp[:, kc % 2, :], AF.Identity, bias=shv[:, kc:kc + 1], scale=wv[:, kc:kc + 1])
            k.pop()

        if stage >= 1:
            mixer_phase(k, T, dict(ident=ident, ones=ones, ones_bf=ones_bf, ident_bf=ident_bf, consts=consts,
                                   epsc=epsc, W1=W1, SH1=SH1), norm_block, ybuf, dump, stage)

        if stage >= 4:
            post_phase(k, nc, T, dict(ident=ident, ones=ones, ones_bf=ones_bf, consts=consts, epsc=epsc,
                                      W2=W2, SH2=SH2, G1=G1, G2=G2, WF=WF, SHF=SHF), norm_block, ybuf, outT, dump)

        fin_ins = [outT]
        if dbg:
            fin_ins.append(dbg_t)
        P.add("sp", lambda e: e.nop(), [], fin_ins)
        cnt = P.emit(nc, stack)
    return nc, cnt


FM = ([("dq%d" % h, h * 128) for h in range(4)] + [("dk%d" % h, 512 + h * 128) for h in range(4)] +
      [("dv%d" % h, 1024 + h * 128) for h in range(4)] + [("mq%d" % i, 2056 + i * 128) for i in range(2)] +
      [("mk%d" % i, 2312 + i * 128) for i in range(2)] + [("dz%d" % h, 1536 + h * 128) for h in range(4)] +
      [("mo%d" % h, 3080 + h * 128) for h in range(4)])
FMI = {n: i for i, (n, _) in enumerate(FM)}
NCONV = 16
BLK = 256


def mixer_phase(k, T, C, norm_block, ybuf, dump, stage):
    ident, ones, ones_bf, consts, epsc = C["ident"], C["ones"], C["ones_bf"], C["consts"], C["epsc"]
    onec = consts[:, C_EPS + 1:C_EPS + 2]
    tri = consts[0:64, C_TRI:C_TRI + 64]
    mneg = consts[0:64, C_MNEG:C_MNEG + 64]
    strict = consts[0:64, C_STRICT:C_STRICT + 64]
    mlow = consts[0:64, C_MLOW:C_MLOW + 64]
    id64 = ident[0:64, 0:64]
    k.push()
    win_bf = k.alloc([8, D_IN], BF16)
    k.push()
    stg = [k.alloc([8, 450], F32) for _ in range(2)]
    for i in range(8):
        s = stg[i % 2]
        k.dma(s, T["win"][:, :, i * 450:(i + 1) * 450], "ws%d" % (i % 2))
        k.copy(win_bf[:, :, i * 450:(i + 1) * 450], s, eng=("act" if i % 2 else "dve"))
    k.pop()
    dnconv = k.alloc([12, 4], F32)
    k.dma(dnconv, T["dnconv"], "c1")
    mlconv = k.alloc([4, 4], F32)
    k.dma(mlconv, T["mlconv"], "c1")
    hp = k.alloc([16], F32)
    k.dma(hp, T["hp"], "c1")
    dnnorm = k.alloc([1], F32)
    k.dma(dnnorm, T["dnnorm"], "c1")
    mlnorm = k.alloc([4], F32)
    k.dma(mlnorm, T["mlnorm"], "c1")
    nega = k.alloc([4], F32)
    k.act(nega, hp[:, 0:4], AF.Exp)
    k.ts(nega, nega, -1.0, None, ALU.mult)

    xb = k.alloc([8, BLK], F32)
    hT = k.alloc([8, BLK], BF16)
    pb = k.alloc([len(FM), 3 + BLK], F32)
    cv = k.alloc([NCONV, BLK], F32)
    cvb = k.alloc([NCONV, BLK], BF16)
    gz = k.alloc([8, BLK], F32)
    ytile = k.alloc([8, BLK], BF16)
    k.memset(pb[:, :, 0:3], 0.0)

    def A(shape, dtp):
        return k.alloc(shape, dtp)
    GD = []
    for h in range(4):
        d = dict(S=A([128], F32), Sb=A([128], BF16), grep=A([128], F32), brep=A([64], F32), Dm=A([64], F32),
                 E=A([64], F32), Bs=A([64], F32), M=A([64], F32), MT=A([64], F32), U=A([64], F32),
                 QK=A([64], BF16), Pa=A([64], F32), PTa=A([64], F32), Pb=A([64], F32), PTb=A([64], F32),
                 kb=A([128], F32), kdec=A([128], BF16), vb=A([128], F32), w=A([128], F32),
                 kcT=A([64], BF16), eGb=A([64], F32), qdT=A([64], BF16), vnew=A([128], BF16),
                 osq=A([64], BF16), rn=A([64], F32), y=A([64], F32))
        k.memset(d["S"], 0.0)
        k.memset(d["Sb"], 0.0)
        GD.append(d)
    ML = []
    for h in range(4):
        d = dict(CN=A([256], F32), CNb=A([256], BF16), ms=A([1], F32), lrep=A([128], F32), nrep=A([128], F32),
                 bb=A([64], F32), nab=A([64], F32), amax=A([1], F32), dcs=A([64], F32), cmx=A([1], F32),
                 mto=A([1], F32), mrep=A([128], F32), tmpm=A([64], F32), D2=A([64], F32), pT=A([64], BF16),
                 inter=A([64], F32), qiT=A([64], BF16), flo=A([64], F32), dn=A([64], F32), hT=A([64], F32),
                 hsq=A([64], BF16), rn=A([64], F32), mx=A([1], F32), keep=A([1], F32), ew=A([1], F32),
                 kw=A([128], BF16))
        k.memset(d["CN"], 0.0)
        k.memset(d["CNb"], 0.0)
        k.memset(d["ms"], 0.0)
        k.memset(d["kw"], 0.0)
        ML.append(d)
    gtm = A([16], F32)
    gt = dict(beta=A([4], F32), x=A([4], F32), ax=A([4], F32), e=A([4], F32), r=A([4], F32), g=A([4], F32),
              G=A([4], F32), Gl=A([4], F32), ebg=A([4], F32), edl=A([4], F32),
              ip=A([4], F32), xf=A([4], F32), lf=A([4], F32), b=A([4], F32), na=A([4], F32))
    vaug = A([4, 256], BF16)
    k.memset(vaug[:, :, 128:256], 1.0)
    ktm = [A([128], F32) for _ in range(2)]
    inv128 = A([128], BF16)
    k.memset(inv128, 1.0 / 128)
    kz = [A([BLK], BF16) for _ in range(4)]
    for h in range(4):
        k.memset(kz[h], 0.0)

    def softplus(out, x, P_=64):
        k.stt(gt["ax"][0:P_], x, -1.0, x, ALU.mult, ALU.max)
        k.act(gt["e"][0:P_], gt["ax"][0:P_], AF.Exp, scale=-1.0)
        k.act(gt["e"][0:P_], gt["e"][0:P_], AF.Ln, bias=onec[0:P_], scale=1.0)
        k.ts(gt["r"][0:P_], x, 0.0, None, ALU.max)
        k.tt(out, gt["r"][0:P_], gt["e"][0:P_], ALU.add)

    nblk = SEQ // BLK if stage >= 3 else 1
    if SIM_NBLK:
        nblk = SIM_NBLK
    for blk in range(nblk):
        t0 = blk * BLK
        if blk > 0:
            k.copy(pb[:, :, 0:3], pb[:, :, BLK:BLK + 3], eng="pool")
        k.dma(xb, T["xT"][:, :, t0:t0 + BLK], "xb")
        norm_block(xb, BLK, C["W1"], C["SH1"], hT)
        if blk == 0:
            dump("hT0", hT[:, 0, :])
        for ci, (nm, c0) in enumerate(FM):
            ps = k.ps(BLK)
            for kc in range(8):
                k.mm(ps, win_bf[:, kc, c0:c0 + 128], hT[:, kc, :], start=(kc == 0), stop=(kc == 7))
            k.copy(pb[:, ci, 3:3 + BLK], ps, eng=("act" if ci % 2 else "dve"))
        if blk == 0:
            dump("pq0", pb[:, FMI["dq0"], 3:3 + BLK])
            dump("mo3", pb[:, FMI["mo3"], 3:3 + BLK])
        if stage < 2:
            continue
        for ci in range(NCONV):
            wv = dnconv[:, ci, :] if ci < 12 else mlconv[:, ci - 12, :]
            k.ts(cv[:, ci, :], pb[:, ci, 0:BLK], wv[:, 0:1], None, ALU.mult, eng="pool")
            for j in range(1, 4):
                k.stt(cv[:, ci, :], pb[:, ci, j:j + BLK], wv[:, j:j + 1], cv[:, ci, :], ALU.mult, ALU.add)
            k.act(cv[:, ci, :], cv[:, ci, :], AF.Silu)
        if SUB == 1:
            dump("cvA", cv[:, 0, :])
            return
        for ci in range(8):
            sq = cvb[:, ci, :]
            k.act(sq, cv[:, ci, :], AF.Square)
            ss = k.ps(BLK)
            k.mm(ss, ones_bf, sq)
            rn = gz[:, 0, :]
            k.act(rn, ss, AF.Sqrt, bias=epsc, scale=1.0)
            k.recip(rn, rn)
            if ci < 4:
                k.stt(cv[:, ci, :], cv[:, ci, :], 128.0 ** -0.5, rn, ALU.mult, ALU.mult)
            else:
                k.tt(cv[:, ci, :], cv[:, ci, :], rn, ALU.mult)
        for ci in (12, 13):
            k.ts(cv[:, ci, :], cv[:, ci, :], 0.125, None, ALU.mult, eng="pool")
        k.copy(cvb, cv, eng="pool")
        for h in range(4):
            p0_ = (h % 2) * 64
            k.copy(kz[h][p0_:p0_ + 64, :], cv[p0_:p0_ + 64, 14 + h // 2, :], eng="pool")
        for h in range(4):
            k.act(gz[:, h, :], pb[:, FMI["dz%d" % h], 3:3 + BLK], AF.Silu)
            k.act(gz[:, 4 + h, :], pb[:, FMI["mo%d" % h], 3:3 + BLK], AF.Sigmoid)
        if blk == 0:
            dump("k0n", cv[:, 4, :])
            dump("mq0", cv[:, 12, :])

        if SUB == 2:
            return
        for j in range(EXP.get("nj", BLK // CH)):
            c0 = j * CH
            cs = slice(c0, c0 + CH)
            gps = k.ps(128)
            for kc in range(8):
                k.mm(gps[0:64, 0:8], hT[:, kc, cs], win_bf[:, kc, 2048:2056], start=(kc == 0), stop=(kc == 7))
            for kc in range(8):
                k.mm(gps[0:64, 8:16], hT[:, kc, cs], win_bf[:, kc, 3592:3600], start=(kc == 0), stop=(kc == 7))
            k.copy(gtm[0:64], gps[0:64, 0:16])
            vps = k.ps(512)
            for kc in range(8):
                k.mm(vps[0:64, :], hT[:, kc, cs], win_bf[:, kc, 2568:3080], start=(kc == 0), stop=(kc == 7))
            k.copy(vaug[0:64, :, 0:128], vps[0:64, :].rearrange("p (h e) -> p h e", h=4), eng="act")
            g = gt
            k.act(g["beta"][0:64], gtm[0:64, 0:4], AF.Sigmoid)
            k.tt(g["x"][0:64], gtm[0:64, 4:8], hp[0:64, 4:8], ALU.add)
            softplus(g["g"][0:64], g["x"][0:64])
            k.tt(g["g"][0:64], g["g"][0:64], nega[0:64], ALU.mult)
            Gps = k.ps(128)
            k.mm(Gps[0:64, 0:4], tri, g["g"][0:64])
            k.mm(Gps[0:64, 4:8], ones[0:64, 0:64], g["g"][0:64])
            k.copy(g["G"][0:64], Gps[0:64, 0:4])
            k.tt(g["edl"][0:64], Gps[0:64, 4:8], g["G"][0:64], ALU.subtract)
            k.act(g["edl"][0:64], g["edl"][0:64], AF.Exp)
            k.act(g["ebg"][0:64], g["G"][0:64], AF.Exp)
            k.tt(g["ebg"][0:64], g["ebg"][0:64], g["beta"][0:64], ALU.mult)
            k.tt(g["ip"][0:64], gtm[0:64, 8:12], hp[0:64, 8:12], ALU.add)
            k.tt(g["xf"][0:64], gtm[0:64, 12:16], hp[0:64, 12:16], ALU.add)
            k.ts(g["xf"][0:64], g["xf"][0:64], -1.0, None, ALU.mult)
            softplus(g["lf"][0:64], g["xf"][0:64])
            k.ts(g["lf"][0:64], g["lf"][0:64], -1.0, None, ALU.mult)
            bps = k.ps(128)
            k.mm(bps[0:64, 0:4], tri, g["lf"][0:64])
            k.copy(g["b"][0:64], bps[0:64, 0:4])
            k.tt(g["na"][0:64], g["ip"][0:64], g["b"][0:64], ALU.subtract)
            if blk == 0 and j == 0:
                dump("G0", g["G"][0:64])
                dump("b0", g["b"][0:64])
                dump("gtm", gtm[0:64])
                dump("hp", hp[0:64])
                dump("ip_a", g["ip"][0:64])
                dump("lf_a", g["lf"][0:64])

            if SUB == 3:
                return
            for pr in range(2):
                tkp = k.ps(128)
                k.tr(tkp[0:64, :], cv[:, 14 + pr, cs], ident)
                k.copy(ktm[pr][0:64], tkp[0:64, :], eng="act")
            def gdn_head(h):
                d = GD[h]
                bank = k.psum[:, h * 512:(h + 1) * 512]
                RA, RB, RC = bank[:, 0:128], bank[:, 128:256], bank[:, 256:512]
                kT, kTb = cv[:, 4 + h, cs], cvb[:, 4 + h, cs]
                qT, qTb = cv[:, h, cs], cvb[:, h, cs]
                vT = cv[:, 8 + h, cs]
                k.copy(d["grep"][0:64], g["g"][0:64, h:h + 1].to_broadcast([64, 128]), eng="pool")
                yield
                k.copy(d["brep"][0:64], g["beta"][0:64, h:h + 1].to_broadcast([64, 64]), eng="pool")
                yield
                Gb = RA
                k.mm(Gb[:, 0:64], d["grep"][0:64], tri)
                yield
                k.mm(Gb[0:64, 64:128], d["brep"][0:64], id64)
                yield
                k.act(d["eGb"], Gb[:, 0:64], AF.Exp)
                yield
                sc = RB
                k.mm(sc[0:64, 0:64], kTb, kTb)
                yield
                k.mm(sc[0:64, 64:128], kTb, qTb)
                yield
                k.stt(d["Dm"][0:64], Gb[0:64, 0:64], g["G"][0:64, h:h + 1], mneg, ALU.subtract, ALU.add)
                yield
                k.act(d["E"][0:64], d["Dm"][0:64], AF.Exp)
                yield
                k.tt(d["Bs"][0:64], Gb[0:64, 64:128], strict, ALU.mult)
                yield
                k.tt(d["M"][0:64], sc[0:64, 0:64], d["E"][0:64], ALU.mult)
                yield
                k.tt(d["M"][0:64], d["M"][0:64], d["Bs"][0:64], ALU.mult)
                yield
                k.tt(d["QK"][0:64], sc[0:64, 64:128], d["E"][0:64], ALU.mult)
                yield
                tp = RC
                k.tr(tp[0:64, 0:64], d["M"][0:64], id64)
                yield
                k.copy(d["MT"][0:64], tp[0:64, 0:64], eng=("dve" if EXP.get("mtdve") else "act"))
                yield
                k.tt(d["U"][0:64], id64, d["M"][0:64], ALU.subtract, eng="pool")
                yield
                Pc, PTc = d["M"], d["MT"]
                bufs = [(d["Pa"], d["PTa"]), (d["Pb"], d["PTb"])]
                for lev in range(1, EXP.get("nlev", 5) + 1):
                    Pn, PTn = bufs[lev % 2]
                    pp = RC
                    if EXP.get("v") == 1:
                        k.mm(pp[0:64, 0:64], Pc[0:64], Pc[0:64])
                        yield
                        k.copy(PTn[0:64], pp[0:64, 0:64])
                        yield
                        return
                    if EXP.get("v") == 2:
                        k.mm(pp[0:64, 0:64], id64, Pc[0:64])
                        yield
                        k.copy(PTn[0:64], pp[0:64, 0:64])
                        yield
                        return
                    if EXP.get("v") == 4:
                        k.mm(pp[0:64, 0:64], Pc[0:64], PTc[0:64])
                        yield
                        k.copy(PTn[0:64], pp[0:64, 0:64])
                        yield
                        return
                    if EXP.get("v") == 6:
                        k.mm(pp[0:64, 0:64], Pc[0:64], PTc[0:64])
                        yield
                        k.mm(pp[0:64, 64:128], PTc[0:64], Pc[0:64])
                        yield
                        k.copy(PTn[0:64], pp[0:64, 0:64])
                        yield
                        k.copy(Pn[0:64], pp[0:64, 64:128])
                        yield
                        return
                    if EXP.get("v") == 7:
                        k.mm(pp[0:64, 0:64], PTc[0:64], Pc[0:64])
                        yield
                        k.copy(PTn[0:64], pp[0:64, 0:64])
                        yield
                        return
                    if EXP.get("v") == 3:
                        k.mm(pp[0:64, 0:64], Pc[0:64], id64)
                        yield
                        k.copy(PTn[0:64], pp[0:64, 0:64])
                        yield
                        return
                    k.mm(pp[0:64, 0:64], Pc[0:64], PTc[0:64])
                    yield
                    if lev < 5:
                        k.mm(pp[0:64, 64:128], PTc[0:64], Pc[0:64])
                        yield
                    k.copy(PTn[0:64], pp[0:64, 0:64], eng="act")
                    yield
                    if lev < 5:
                        k.copy(Pn[0:64], pp[0:64, 64:128])
                        yield
                    if EXP.get("noup"):
                        Pc, PTc = Pn, PTn
                        continue
                    up = RA
                    k.mm(up[0:64, 0:64], PTn[0:64], d["U"][0:64])
                    yield
                    k.tt(d["U"][0:64], d["U"][0:64], up[0:64, 0:64], ALU.add)
                    yield
                    Pc, PTc = Pn, PTn
                tk = RC
                k.tr(tk[0:64, 0:128], kT, ident)
                yield
                k.tr(tk[0:64, 128:256], vT, ident)
                yield
                k.ts(d["kb"][0:64], tk[0:64, 0:128], g["ebg"][0:64, h:h + 1], None, ALU.mult)
                yield
                k.ts(d["kdec"][0:64], tk[0:64, 0:128], g["edl"][0:64, h:h + 1], None, ALU.mult)
                yield
                k.ts(d["vb"][0:64], tk[0:64, 128:256], g["beta"][0:64, h:h + 1], None, ALU.mult)
                yield
                wk = RC
                k.mm(wk[0:64, 0:128], d["U"][0:64], d["vb"][0:64])
                yield
                k.mm(wk[:, 128:192], d["kb"][0:64], d["U"][0:64])
                yield
                k.copy(d["w"][0:64], wk[0:64, 0:128], eng="act")
                yield
                k.copy(d["kcT"], wk[:, 128:192])
                yield
                k.tt(d["qdT"], qT, d["eGb"], ALU.mult, eng="pool")
                yield
                p1 = RA
                k.mm(p1[0:64, :], d["kcT"], d["Sb"])
                yield
                k.tt(d["vnew"][0:64], d["w"][0:64], p1[0:64, :], ALU.subtract)
                yield
                op_ = RB
                k.mm(op_[:, 0:64], d["Sb"], d["qdT"], start=True, stop=False)
                yield
                k.mm(op_[:, 0:64], d["vnew"][0:64], d["QK"][0:64], start=False, stop=True)
                yield
                dS = RC[:, 0:128]
                k.mm(dS, d["kdec"][0:64], d["vnew"][0:64])
                yield
                k.stt(d["S"], d["S"], d["eGb"][:, 63:64], dS, ALU.mult, ALU.add)
                yield
                k.copy(d["Sb"], d["S"], eng="act")
                yield
                k.copy(d["y"], op_[:, 0:64], eng="act")
                yield
                k.tt(d["osq"], d["y"], d["y"], ALU.mult, eng="pool")
                yield
                ss = RA
                k.mm(ss[:, 0:64], inv128, d["osq"])
                yield
                k.act(d["rn"], ss[:, 0:64], AF.Ln, bias=epsc, scale=1.0)
                yield
                k.act(d["rn"], d["rn"], AF.Exp, scale=-0.5)
                yield
                k.stt(d["y"], d["y"], dnnorm[:, 0:1], d["rn"], ALU.mult, ALU.mult)
                yield
                k.tt(ytile[:, h, cs], d["y"], gz[:, h, cs], ALU.mult, eng="pool")
                yield
                if blk == 0 and j == 0 and h == 0:
                    dump("M0", d["M"][0:64])
                    dump("U0", d["U"][0:64])
                    dump("y0", d["y"])

            def ml_head(h):
                d = ML[h]
                bank = k.psum[:, (4 + h) * 512:(5 + h) * 512]
                RA, RB, RC = bank[:, 0:128], bank[:, 128:256], bank[:, 256:512]
                pr, p0 = h // 2, (h % 2) * 64
                psl = slice(p0, p0 + 64)
                qT = cv[psl, 12 + pr, cs]
                qTb, kTb = cvb[psl, 12 + pr, cs], cvb[psl, 14 + pr, cs]
                k.copy(d["lrep"][0:64], g["lf"][0:64, h:h + 1].to_broadcast([64, 128]), eng="pool")
                yield
                k.copy(d["nrep"][0:64], g["na"][0:64, h:h + 1].to_broadcast([64, 128]), eng="pool")
                yield
                bp = RA
                k.mm(bp[:, 0:64], d["lrep"][0:64], tri)
                yield
                k.mm(bp[:, 64:128], d["nrep"][0:64], id64)
                yield
                k.copy(d["bb"], bp[:, 0:64], eng="act")
                yield
                k.copy(d["nab"], bp[:, 64:128])
                yield
                k.rmax(d["amax"], d["nab"])
                yield
                k.tt(d["dcs"][0:64], d["nab"][0:64], mlow, ALU.add, eng="pool")
                yield
                k.rmax(d["cmx"][0:64], d["dcs"][0:64])
                yield
                k.tt(d["mto"][0:64], d["cmx"][0:64], d["ms"][0:64], ALU.max)
                yield
                k.copy(d["mrep"][0:64], d["mto"][0:64, 0:1].to_broadcast([64, 128]), eng="pool")
                yield
                mp = RB
                k.mm(mp[:, 0:64], d["mrep"][0:64], id64)
                yield
                sc = RC
                k.mm(sc[0:64, 0:64], kz[h][:, cs], cvb[:, 12 + pr, cs])
                yield
                k.ts(d["tmpm"][0:64], mneg, g["na"][0:64, h:h + 1], None, ALU.add, eng="pool")
                yield
                k.stt(d["D2"][0:64], mp[0:64, 0:64], -1.0, d["tmpm"][0:64], ALU.mult, ALU.add)
                yield
                k.act(d["D2"][0:64], d["D2"][0:64], AF.Exp)
                yield
                k.tt(d["pT"][0:64], sc[0:64, 0:64], d["D2"][0:64], ALU.mult)
                yield
                k.act(d["inter"], mp[:, 0:64], AF.Exp, bias=d["ms"], scale=-1.0)
                yield
                k.tt(d["qiT"], cv[:, 12 + pr, cs], d["inter"], ALU.mult, eng="pool")
                yield
                k.tt(d["flo"], d["bb"], mp[:, 0:64], ALU.add)
                yield
                k.act(d["flo"], d["flo"], AF.Exp, scale=-1.0)
                yield
                nd = RC[:, 0:128]
                k.mm(nd[:, 0:64], d["CNb"][:, 0:128], d["qiT"], start=True, stop=False)
                yield
                k.mm(nd[:, 0:64], vaug[0:64, h, 0:128], d["pT"][0:64], start=False, stop=True)
                yield
                k.mm(nd[:, 64:128], d["CNb"][:, 128:256], d["qiT"], start=True, stop=False)
                yield
                k.mm(nd[:, 64:128], vaug[0:64, h, 128:256], d["pT"][0:64], start=False, stop=True)
                yield
                k.ts(d["dn"], nd[:, 64:128], -1.0, None, ALU.mult)
                yield
                k.tt(d["dn"], d["dn"], nd[:, 64:128], ALU.max)
                yield
                k.tt(d["dn"], d["dn"], d["flo"], ALU.max)
                yield
                k.recip(d["dn"], d["dn"])
                yield
                k.tt(d["hT"], nd[:, 0:64], d["dn"], ALU.mult)
                yield
                k.tt(d["hsq"], d["hT"], d["hT"], ALU.mult, eng="pool")
                yield
                ss = RA
                k.mm(ss[:, 0:64], inv128, d["hsq"])
                yield
                k.act(d["rn"], ss[:, 0:64], AF.Ln, bias=epsc, scale=1.0)
                yield
                k.act(d["rn"], d["rn"], AF.Exp, scale=-0.5)
                yield
                k.stt(d["hT"], d["hT"], mlnorm[:, h:h + 1], d["rn"], ALU.mult, ALU.mult)
                yield
                k.tt(ytile[:, 4 + h, cs], d["hT"], gz[:, 4 + h, cs], ALU.mult, eng="pool")
                yield
                k.tt(d["mx"], d["ms"], d["amax"], ALU.max)
                yield
                k.tt(d["keep"], d["ms"], d["mx"], ALU.subtract, eng="pool")
                yield
                k.act(d["keep"], d["keep"], AF.Exp)
                yield
                k.tt(d["ew"][0:64], g["na"][0:64, h:h + 1], d["mx"][0:64], ALU.subtract)
                yield
                k.act(d["ew"][0:64], d["ew"][0:64], AF.Exp)
                yield
                k.ts(d["kw"][0:64, p0:p0 + 64], ktm[pr][0:64, p0:p0 + 64], d["ew"][0:64, 0:1], None, ALU.mult)
                yield
                dc = RC
                k.mm(dc, d["kw"][0:64], vaug[0:64, h, :])
                yield
                k.stt(d["CN"], d["CN"], d["keep"][:, 0:1], dc, ALU.mult, ALU.add)
                yield
                k.copy(d["CNb"], d["CN"], eng="act")
                yield
                k.tt(d["ms"], d["bb"][:, 63:64], d["mx"], ALU.add, eng="pool")
                yield
                if blk == 0 and j == 0 and h == 0:
                    dump("mh0", d["hT"])
                    dump("nab", d["nab"])
                    dump("lrep", d["lrep"][0:64])
                    dump("nrep", d["nrep"][0:64])
                    dump("lf", g["lf"][0:64])
                    dump("na", g["na"][0:64])
                    dump("ip", g["ip"][0:64])
                    dump("bb", d["bb"])
                    dump("inter", d["inter"])
                    dump("flo", d["flo"])
                    dump("dn", d["dn"])
                    dump("pT", d["pT"][0:64])
                    dump("qiT", d["qiT"][0:64])
                    dump("rnm", d["rn"])

            gens = [gdn_head(h) for h in range(4)] + [ml_head(h) for h in range(4)]
            while gens:
                for g_ in list(gens):
                    try:
                        next(g_)
                    except StopIteration:
                        gens.remove(g_)
        if SUB == 5:
            return
        for hh in range(0 if EXP.get("noyb") else 8):
            half, off = t0 // 2048, t0 % 2048
            r0 = (half * 8 + hh) * 128
            k.dma(ybuf[r0:r0 + 128, off:off + BLK], ytile[:, hh, :], "yb")
        if blk == 0:
            k.push()
            yd = k.alloc([BLK], F32)
            for hh in (0, 4):
                k.copy(yd, ytile[:, hh, :])
                dump("yt%d" % hh, yd)
            k.pop()
    k.pop()


def _kc(a):
    a = np.asarray(a, np.float32)
    return np.ascontiguousarray(a.reshape((8, 128) + a.shape[1:]).swapaxes(0, 1))


def prep_inputs(inp):
    f = np.float32
    x = np.asarray(inp["x"], f)
    consts = make_consts()
    w_all = np.concatenate([np.asarray(inp["w_ada"][0], f), np.asarray(inp["w_ada_final"], f)], axis=1)
    b_all = np.concatenate([np.asarray(inp["b_ada"][0], f), np.asarray(inp["b_ada_final"], f)])
    shared = {
        "wada": _kc(w_all),
        "bada": np.ascontiguousarray(b_all.reshape(64, 128).T),
        "norms": np.ascontiguousarray(np.concatenate(
            [np.asarray(inp[n], f).reshape(8, 128).T for n in ("norm_mix", "norm_ffn", "norm_final")], axis=1)),
        "win": _kc(inp["w_in"][0]),
        "dnconv": np.ascontiguousarray(np.asarray(inp["dn_conv"][0], f).reshape(4, 12, 128).transpose(2, 1, 0)),
        "mlconv": np.ascontiguousarray(np.asarray(inp["ml_conv"][0], f).reshape(4, 4, 128).transpose(2, 1, 0)),
        "hp": np.ascontiguousarray(np.broadcast_to(np.concatenate(
            [np.asarray(inp[n][0], f) for n in ("dn_a_log", "dn_dt_bias", "ml_i_bias", "ml_f_bias")])[None, :], (128, 16))),
        "dnnorm": np.ascontiguousarray(np.asarray(inp["dn_norm"][0], f).reshape(128, 1)),
        "mlnorm": np.ascontiguousarray(np.asarray(inp["ml_norm"][0], f).reshape(4, 128).T),
        "wout": _kc(inp["w_out"][0]),
        "wr": _kc(inp["w_router"][0]),
        "br": np.ascontiguousarray(np.broadcast_to(np.asarray(inp["b_router"][0], f)[None, :], (128, 32))),
        "wgu": np.ascontiguousarray(np.asarray(inp["w_gate_up"][0], f)),
        "bgu": np.ascontiguousarray(np.asarray(inp["b_gate_up"][0], f).reshape(32, 16, 128).transpose(2, 0, 1)),
        "wd": np.ascontiguousarray(np.asarray(inp["w_down"][0], f)),
        "bd": np.ascontiguousarray(np.asarray(inp["b_down"][0], f)),
        "consts": consts,
    }
    maps = []
    for c in range(8):
        b, hf = c // 2, c % 2
        xT = _kc(x[b].T)
        m = dict(shared)
        m["xT"] = xT
        m["xo"] = np.ascontiguousarray(xT[:, :, hf * 2048:(hf + 1) * 2048])
        m["cT"] = np.ascontiguousarray(np.asarray(inp["c"], f)[b].reshape(8, 128).T)
        p = np.arange(128, dtype=np.int32)[:, None]
        h = np.arange(8, dtype=np.int32)[None, :]
        m["gidx"] = np.ascontiguousarray(((hf * 8 + h) * 128 + p).astype(np.int32))
        maps.append(m)
    return maps


def post_phase(k, nc, T, C, norm_block, ybuf, outT, dump):
    ident, epsc = C["ident"], C["epsc"]
    P = k.P
    P.whole.add("ybuf")
    NT = 2048
    TB = 512
    k.push()
    gidx = k.alloc([8], I32)
    k.dma(gidx, T["gidx"], "c2")
    h2T = k.alloc([8, NT], BF16)
    for hh in range(8):
        P.add("pool", (lambda hh: lambda e: e.indirect_dma_start(
            out=h2T[:, hh, :], out_offset=None, in_=ybuf[:, :],
            in_offset=bass.IndirectOffsetOnAxis(ap=gidx[:, hh:hh + 1], axis=0)))(hh),
            [h2T[:, hh, :]], [ybuf, gidx[:, hh:hh + 1]], dma_key="yg")
    if PSUB == 1:
        k.push()
        yd_ = k.alloc([512], F32)
        k.copy(yd_, h2T[:, 0, 0:512])
        dump("yg0", yd_)
        k.pop()
        return
    GT = k.alloc([NT], F32)
    wr = k.alloc([8, 32], F32)
    k.dma(wr, T["wr"], "c2")
    br = k.alloc([32], F32)
    k.dma(br, T["br"], "c2")
    bd = k.alloc([1024], F32)
    k.dma(bd[0:32], T["bd"], "c2")
    bgu = k.alloc([32, 16], F32)
    k.dma(bgu, T["bgu"], "c2")
    stg = [k.alloc([2048], F32) for _ in range(2)]
    sti = [0]

    def load_cast(dst_bf, src, n):
        s_ = stg[sti[0] % 2]
        key = "st%d" % (sti[0] % 2)
        sti[0] += 1
        k.dma(s_[:, 0:n], src, key)
        k.copy(dst_bf, s_[:, 0:n], eng="act")

    k.push()
    wout_bf = k.alloc([8, 1024], BF16)
    for yc in range(8):
        load_cast(wout_bf[:, yc, :], T["wout"][:, yc, :], 1024)
    xob = k.alloc([8, TB], F32)
    x1b = k.alloc([8, TB], F32)
    h2f = k.alloc([8, TB], F32)
    lg = k.alloc([32], F32)
    mx8 = k.alloc([8], F32)
    nm1 = k.alloc([1], F32)
    msk = k.alloc([32], F32)
    ex = k.alloc([32], F32)
    den = k.alloc([1], F32)
    for tb in range(NT // TB):
        ts_ = slice(tb * TB, (tb + 1) * TB)
        k.dma(xob, T["xo"][:, :, ts_], "xo")
        for dc in range(8):
            ps = k.ps(TB)
            for yc in range(8):
                k.mm(ps, wout_bf[:, yc, dc * 128:(dc + 1) * 128], h2T[:, yc, ts_], start=(yc == 0), stop=(yc == 7))
            k.stt(x1b[:, dc, :], ps, C["G1"][:, dc:dc + 1], xob[:, dc, :], ALU.mult, ALU.add)
        k.dma(T["x1buf"][:, :, ts_], x1b, "x1w")
        if tb == 0:
            dump("x1", x1b[:, 0, :])
        norm_block(x1b, TB, C["W2"], C["SH2"], h2f)
        k.copy(h2T[:, :, ts_], h2f, eng="pool")
        for tt_ in range(TB // 128):
            tsl = slice(tt_ * 128, (tt_ + 1) * 128)
            lp = k.ps(128)
            for kc in range(8):
                k.mm(lp[:, 0:32], h2f[:, kc, tsl], wr[:, kc, :], start=(kc == 0), stop=(kc == 7))
            k.tt(lg, lp[:, 0:32], br, ALU.add)
            P.add("dve", lambda e: e.max(mx8, lg), [mx8], [lg])
            k.ts(msk, lg, mx8[:, 3:4], None, ALU.is_ge)
            k.ts(nm1, mx8[:, 0:1], -1.0, None, ALU.mult)
            k.act(ex, lg, AF.Exp, bias=nm1, scale=1.0)
            k.tt(ex, ex, msk, ALU.mult)
            k.rsum(den, ex)
            k.recip(den, den)
            k.ts(ex, ex, den[:, 0:1], None, ALU.mult)
            tp = k.ps(128)
            k.tr(tp[0:32, 0:128], ex, ident)
            g0 = tb * TB + tt_ * 128
            k.copy(GT[0:32, g0:g0 + 128], tp[0:32, 0:128], eng="act")
            if tb == 0 and tt_ == 0:
                dump("lg", lg)
                dump("gate", ex)
    k.pop()
    if PSUB == 2:
        return

    acc = k.alloc([8, NT], F32)
    k.push()
    wgu_bf = k.alloc([8, 2048], BF16)
    wd_bf = k.alloc([8, 1024], BF16)
    sel = k.alloc([128], F32)
    gbe = k.alloc([TB], F32)
    actT = k.alloc([8, TB], BF16)
    tg = [k.alloc([TB], F32) for _ in range(2)]
    tsg = [k.alloc([TB], F32) for _ in range(2)]
    tu = [k.alloc([TB], F32) for _ in range(2)]
    for tb in range(NT // TB):
        ts_ = slice(tb * TB, (tb + 1) * TB)
        for dc in range(8):
            ps = k.ps(TB)
            k.mm(ps, bd[0:32, dc * 128:(dc + 1) * 128], GT[0:32, ts_])
            k.copy(acc[:, dc, ts_], ps, eng=("act" if dc % 2 else "dve"))
    if EXP.get("m") == 0:
        return
    for kc in range(8):
        load_cast(wgu_bf[:, kc, :], T["wgu"][0, kc * 128:(kc + 1) * 128, :], 2048)
    if EXP.get("m") == 1:
        return
    NE = SIM_NE or 32
    it = 0
    for e in range(NE):
        for fc in range(8):
            load_cast(wd_bf[:, fc, :], T["wd"][e, fc * 128:(fc + 1) * 128, :], 1024)
        k.copy(sel[0:32], ident[0:32, e:e + 1].to_broadcast([32, 128]))
        for tb in range(NT // TB):
            ts_ = slice(tb * TB, (tb + 1) * TB)
            gp = k.ps(TB)
            k.mm(gp, sel[0:32], GT[0:32, ts_])
            k.copy(gbe, gp, eng="act")
            if EXP.get("m") == 2:
                return
            for fc in range(8):
                b = it % 2
                it += 1
                gps = k.ps(TB)
                for kc in range(8):
                    k.mm(gps, wgu_bf[:, kc, fc * 128:(fc + 1) * 128], h2T[:, kc, ts_], start=(kc == 0), stop=(kc == 7))
                ups = k.ps(TB)
                for kc in range(8):
                    k.mm(ups, wgu_bf[:, kc, 1024 + fc * 128:1024 + (fc + 1) * 128], h2T[:, kc, ts_],
                         start=(kc == 0), stop=(kc == 7))
                k.ts(tg[b], gps, bgu[:, e, fc:fc + 1], 7.0, ALU.add, ALU.min)
                k.act(tsg[b], tg[b], AF.Sigmoid, scale=1.702)
                k.ts(tu[b], ups, bgu[:, e, 8 + fc:9 + fc], 7.0, ALU.add, ALU.min)
                if EXP.get("m") == 3:
                    return
                k.ts(tu[b], tu[b], -7.0, 1.0, ALU.max, ALU.add)
                k.tt(tg[b], tg[b], tsg[b], ALU.mult)
                k.tt(tu[b], tu[b], gbe, ALU.mult, eng="pool")
                k.tt(actT[:, fc, :], tg[b], tu[b], ALU.mult, eng="pool")
                if EXP.get("m") == 4:
                    return
            if EXP.get("m") == 5:
                return
            if e + 1 < NE and tb == NT // TB - 1:
                for kc in range(8):
                    load_cast(wgu_bf[:, kc, :], T["wgu"][e + 1, kc * 128:(kc + 1) * 128, :], 2048)
            for dc in range(8):
                ps = k.ps(TB)
                for fc in range(8):
                    k.mm(ps, wd_bf[:, fc, dc * 128:(dc + 1) * 128], actT[:, fc, :], start=(fc == 0), stop=(fc == 7))
                k.tt(acc[:, dc, ts_], acc[:, dc, ts_], ps, ALU.add)
            if EXP.get("m") == 6:
                return
            if EXP.get("m") == 7 and tb == 1:
                return
            if EXP.get("m") == 8 and tb == 3:
                dump("acc", acc[:, 0, 0:512])
                return
    dump("acc", acc[:, 0, 0:512])
    k.pop()
    if PSUB == 3:
        return

    k.push()
    x1b = k.alloc([8, TB], F32)
    of = k.alloc([8, TB], F32)
    for tb in range(NT // TB):
        ts_ = slice(tb * TB, (tb + 1) * TB)
        k.dma(x1b, T["x1buf"][:, :, ts_], "x1r")
        for dc in range(8):
            k.stt(x1b[:, dc, :], acc[:, dc, ts_], C["G2"][:, dc:dc + 1], x1b[:, dc, :], ALU.mult, ALU.add)
        norm_block(x1b, TB, C["WF"], C["SHF"], of)
        k.dma(outT[:, :, ts_], of, "out")
    k.pop()
    k.pop()


_CACHE = {}


def kernel(**inputs):
    if "nc" not in _CACHE:
        _CACHE["nc"] = build(stage=99, dbg=False)[0]
    nc = _CACHE["nc"]
    maps = prep_inputs(inputs)
    res = run_bass_kernel_spmd(nc, maps, core_ids=list(range(8)))
    out = np.zeros((NB, SEQ, D), np.float32)
    for c in range(8):
        b, hf = c // 2, c % 2
        o = np.asarray(res.results[c]["outT"])
        out[b, hf * 2048:(hf + 1) * 2048, :] = o.transpose(2, 1, 0).reshape(2048, D)
    return out
```
